# Optimizing a Trainium2 kernel written in Bass

```python
import jax
import jax.numpy as jnp
from jax import lax
import numpy as np

D_MODEL = 1024
BATCH = 4
SEQ = 8192
DEPTH = 1

GRID_W = 64
CTX_LEN = 256
D_MIX = D_MODEL
D_MLSTM = D_MIX // 2
D_CONV = D_MIX - D_MLSTM
M_HEADS = 4
V_DIM = D_MLSTM // M_HEADS
QK_DIM = V_DIM // 2
D_QK = M_HEADS * QK_DIM
N_GATES = 4 * M_HEADS
CHUNK = 128
CONV_WIDTH = 31
N_EXPERTS = 64
TOP_K = 8
D_EXPERT = D_MODEL // 4
D_SHARED = D_EXPERT
ROUTE_SCALE = 2.5
EXPERT_BLOCK = 128
EPS = 1e-6
IN_SPLITS = (D_QK, 2 * D_QK, 2 * D_QK + D_MLSTM, 2 * D_QK + 2 * D_MLSTM,
             2 * D_QK + 2 * D_MLSTM + N_GATES, 2 * D_QK + 2 * D_MLSTM + N_GATES + D_CONV)
D_IN = 2 * D_QK + 2 * D_MLSTM + N_GATES + 2 * D_CONV

kernel_name = "hybrid_mlstm_conformer_moe_dit_layer"


def _rms_norm(x, g):
    xf = x.astype(jnp.float32)
    y = xf * lax.rsqrt(jnp.mean(xf * xf, axis=-1, keepdims=True) + EPS)
    return (y * g.astype(jnp.float32)).astype(x.dtype)


def _layer_norm(x, g, b):
    xf = x.astype(jnp.float32)
    mu = jnp.mean(xf, axis=-1, keepdims=True)
    xc = xf - mu
    y = xc * lax.rsqrt(jnp.mean(xc * xc, axis=-1, keepdims=True) + EPS)
    return (y * g.astype(jnp.float32) + b.astype(jnp.float32)).astype(x.dtype)


def _modulate(h, shift, scale):
    return h * (1.0 + scale) + shift


def _heads(t, n):
    B, T, _ = t.shape
    return t.reshape(B, T, n, -1).transpose(0, 2, 1, 3).astype(jnp.float32)


def _mlstm_chunk_states(k, v, log_i, log_f, state0):
    B, H, T, dk = k.shape
    dv = v.shape[-1]
    nc = T // CHUNK
    kc = k.reshape(B, H, nc, CHUNK, dk)
    vc = v.reshape(B, H, nc, CHUNK, dv)
    li = log_i.reshape(B, H, nc, CHUNK)
    b = jnp.cumsum(log_f.reshape(B, H, nc, CHUNK), axis=-1)
    b_tot = b[..., -1]
    g = b_tot[..., None] - b + li
    m_loc = jnp.max(g, axis=-1)
    w = jnp.exp(g - m_loc[..., None])
    C_loc = jnp.einsum('bhcs,bhcsv,bhcsk->bhcvk', w, vc, kc)
    n_loc = jnp.einsum('bhcs,bhcsk->bhck', w, kc)

    def step(state, xs):
        C, n, m = state
        bt, ml, Cl, nl = xs
        m_new = jnp.maximum(bt + m, ml)
        a = jnp.exp(bt + m - m_new)
        e = jnp.exp(ml - m_new)
        C_new = a[..., None, None] * C + e[..., None, None] * Cl
        n_new = a[..., None] * n + e[..., None] * nl
        return (C_new, n_new, m_new), (C, n, m)

    xs = (jnp.moveaxis(b_tot, 2, 0), jnp.moveaxis(m_loc, 2, 0),
          jnp.moveaxis(C_loc, 2, 0), jnp.moveaxis(n_loc, 2, 0))
    final, prev = lax.scan(step, state0, xs)
    prev = (jnp.moveaxis(prev[0], 0, 2), jnp.moveaxis(prev[1], 0, 2), jnp.moveaxis(prev[2], 0, 2))
    return prev, final


def _mlstm_chunk_outputs(q, k, v, log_i, log_f, prev):
    C_prev, n_prev, m_prev = prev
    B, H, T, dk = q.shape
    dv = v.shape[-1]
    nc = T // CHUNK
    qc = q.reshape(B, H, nc, CHUNK, dk)
    kc = k.reshape(B, H, nc, CHUNK, dk)
    vc = v.reshape(B, H, nc, CHUNK, dv)
    li = log_i.reshape(B, H, nc, CHUNK)
    b = jnp.cumsum(log_f.reshape(B, H, nc, CHUNK), axis=-1)
    lower = jnp.tril(jnp.ones((CHUNK, CHUNK), dtype=bool))
    log_d = jnp.where(lower, b[..., :, None] - b[..., None, :] + li[..., None, :], -jnp.inf)
    a = b + m_prev[..., None]
    m_t = jnp.maximum(a, jnp.max(log_d, axis=-1))
    s = jnp.einsum('bhctk,bhcsk->bhcts', qc, kc) * jnp.exp(log_d - m_t[..., None])
    e_int = jnp.exp(a - m_t)
    num = (jnp.einsum('bhcts,bhcsv->bhctv', s, vc)
           + e_int[..., None] * jnp.einsum('bhcvk,bhctk->bhctv', C_prev, qc))
    den = jnp.sum(s, axis=-1) + e_int * jnp.einsum('bhck,bhctk->bhct', n_prev, qc)
    h = num / jnp.maximum(jnp.abs(den), jnp.exp(-m_t))[..., None]
    return h.reshape(B, H, T, dv)


def _mlstm_direction(q, k, v, log_i, log_f, state0, reverse, want_out):
    if reverse:
        q, k, v = jnp.flip(q, 2), jnp.flip(k, 2), jnp.flip(v, 2)
        log_i, log_f = jnp.flip(log_i, 2), jnp.flip(log_f, 2)
    prev, final = _mlstm_chunk_states(k, v, log_i, log_f, state0)
    h = None
    if want_out:
        h = _mlstm_chunk_outputs(q, k, v, log_i, log_f, prev)
        if reverse:
            h = jnp.flip(h, 2)
    return h, final


def _mlstm_bidir(q, k, v, gates, states0, want_out):
    h_f, s_f = _mlstm_direction(q, k, v, gates[:, 0], jax.nn.log_sigmoid(gates[:, 1]),
                                states0[0], False, want_out)
    h_b, s_b = _mlstm_direction(q, k, v, gates[:, 2], jax.nn.log_sigmoid(gates[:, 3]),
                                states0[1], True, want_out)
    h = h_f + h_b if want_out else None
    return h, (s_f, s_b)


def _project(h, w_in, b_in, b_gate):
    B, T, _ = h.shape
    z = h @ w_in + b_in
    q, k, v, o, gt, ga, gb = jnp.split(z, IN_SPLITS, axis=-1)
    q = _heads(q, M_HEADS)
    k = _heads(k, M_HEADS) * (QK_DIM ** -0.5)
    v = _heads(v, M_HEADS)
    gates = (gt.reshape(B, T, 4, M_HEADS).transpose(0, 2, 3, 1).astype(jnp.float32)
             + b_gate.astype(jnp.float32)[None, :, :, None])
    u = ga * jax.nn.sigmoid(gb)
    return (q, k, v, gates), o, u


def _mlstm_group_out(h, o, norm_g):
    B, H, T, dv = h.shape
    hn = h * lax.rsqrt(jnp.mean(h * h, axis=-1, keepdims=True) + EPS) * norm_g.astype(jnp.float32)[:, None, :]
    return hn.transpose(0, 2, 1, 3).reshape(B, T, H * dv).astype(o.dtype) * jax.nn.sigmoid(o)


def _conv_group_out(u, conv_w, conv_b, ln_g, ln_b, rows):
    B, T, C = u.shape
    lhs = u.reshape(B * rows, T // rows, C)
    y = lax.conv_general_dilated(lhs, conv_w.astype(u.dtype)[:, None, :], (1,), 'SAME',
                                 dimension_numbers=('NWC', 'WIO', 'NWC'), feature_group_count=C)
    y = y.reshape(B, T, C) + conv_b
    return jax.nn.silu(_layer_norm(y, ln_g, ln_b))


def _token_mixer(hx, hc, w_in, b_in, b_gate, norm_g, conv_w, conv_b, ln_g, ln_b, w_out, b_out, ctx_out):
    B = hx.shape[0]
    rows = hx.shape[1] // GRID_W
    mx_in, ox, ux = _project(hx, w_in, b_in, b_gate)
    mc_in, oc, uc = _project(hc, w_in, b_in, b_gate)
    zero = (jnp.zeros((B, M_HEADS, V_DIM, QK_DIM), jnp.float32),
            jnp.zeros((B, M_HEADS, QK_DIM), jnp.float32),
            jnp.zeros((B, M_HEADS), jnp.float32))
    hc_m, ctx_states = _mlstm_bidir(*mc_in, (zero, zero), ctx_out)
    hx_m, _ = _mlstm_bidir(*mx_in, ctx_states, True)
    yx = jnp.concatenate([_mlstm_group_out(hx_m, ox, norm_g),
                          _conv_group_out(ux, conv_w, conv_b, ln_g, ln_b, rows)], axis=-1) @ w_out + b_out
    yc = None
    if ctx_out:
        yc = jnp.concatenate([_mlstm_group_out(hc_m, oc, norm_g),
                              _conv_group_out(uc, conv_w, conv_b, ln_g, ln_b, 1)], axis=-1) @ w_out + b_out
    return yx, yc


def _moe_ffn(h, w_router, b_router, we_gate, we_up, we_down, ws_gate, ws_up, ws_down):
    B, T, D = h.shape
    hf = h.reshape(-1, D)
    nt = hf.shape[0]
    s = jax.nn.sigmoid((hf @ w_router).astype(jnp.float32))
    _, idx = lax.top_k(s + b_router.astype(jnp.float32), TOP_K)
    s_sel = jnp.take_along_axis(s, idx, axis=-1)
    wts = s_sel / jnp.sum(s_sel, axis=-1, keepdims=True) * ROUTE_SCALE
    n_assign = nt * TOP_K
    e_flat = idx.reshape(-1)
    tok_flat = jnp.repeat(jnp.arange(nt, dtype=jnp.int32), TOP_K)
    order = jnp.argsort(e_flat)
    e_s, tok_s, w_s = e_flat[order], tok_flat[order], wts.reshape(-1)[order]
    counts = jnp.bincount(e_flat, length=N_EXPERTS)
    starts = jnp.cumsum(counts) - counts
    padded = (counts + EXPERT_BLOCK - 1) // EXPERT_BLOCK * EXPERT_BLOCK
    pend = jnp.cumsum(padded)
    pstarts = pend - padded
    dest = pstarts[e_s] + (jnp.arange(n_assign, dtype=jnp.int32) - starts[e_s])
    n_rows = -(-(n_assign + N_EXPERTS * (EXPERT_BLOCK - 1)) // EXPERT_BLOCK) * EXPERT_BLOCK
    n_blocks = n_rows // EXPERT_BLOCK
    row_tok = jnp.full((n_rows,), nt, jnp.int32).at[dest].set(tok_s)
    row_w = jnp.zeros((n_rows,), jnp.float32).at[dest].set(w_s)
    block_e = jnp.minimum(jnp.searchsorted(pend, jnp.arange(n_blocks) * EXPERT_BLOCK, side='right'),
                          N_EXPERTS - 1)
    hpad = jnp.concatenate([hf, jnp.zeros((1, D), hf.dtype)], axis=0)

    def body(acc, xs):
        rows, rw, e = xs
        xb = hpad[rows]
        y = (jax.nn.silu(xb @ we_gate[e]) * (xb @ we_up[e])) @ we_down[e]
        return acc.at[rows].add(y * rw[:, None].astype(y.dtype)), None

    acc, _ = lax.scan(body, jnp.zeros((nt + 1, D), hf.dtype),
                      (row_tok.reshape(n_blocks, EXPERT_BLOCK), row_w.reshape(n_blocks, EXPERT_BLOCK), block_e))
    shared = (jax.nn.silu(hf @ ws_gate) * (hf @ ws_up)) @ ws_down
    return (acc[:nt] + shared).reshape(B, T, D)


def setup_inputs(seed: int = 0) -> dict:
    key = jax.random.key(seed)
    ks = jax.random.split(key, 32)

    def nrm(k, shape, scale):
        return jax.random.normal(k, shape, jnp.float32) * scale

    D = D_MODEL
    b_gate = jnp.stack([nrm(ks[12], (DEPTH, M_HEADS), 0.1),
                        3.0 + 3.0 * jax.random.uniform(ks[13], (DEPTH, M_HEADS), jnp.float32),
                        nrm(ks[14], (DEPTH, M_HEADS), 0.1),
                        3.0 + 3.0 * jax.random.uniform(ks[15], (DEPTH, M_HEADS), jnp.float32)], axis=1)
    return {
        "x": nrm(ks[0], (BATCH, SEQ, D), 1.0),
        "c": nrm(ks[1], (BATCH, D), 1.0),
        "ctx": nrm(ks[2], (BATCH, CTX_LEN, D), 1.0),
        "c_ctx": nrm(ks[3], (D,), 1.0),
        "w_ada": nrm(ks[4], (DEPTH, D, 6 * D), 0.5 * D ** -0.5),
        "b_ada": nrm(ks[5], (DEPTH, 6 * D), 0.02),
        "g_pre_mix": 1.0 + nrm(ks[6], (DEPTH, D), 0.1),
        "g_post_mix": 1.0 + nrm(ks[7], (DEPTH, D), 0.1),
        "g_pre_ffn": 1.0 + nrm(ks[8], (DEPTH, D), 0.1),
        "g_post_ffn": 1.0 + nrm(ks[9], (DEPTH, D), 0.1),
        "w_in": nrm(ks[10], (DEPTH, D, D_IN), D ** -0.5),
        "b_in": nrm(ks[11], (DEPTH, D_IN), 0.02),
        "b_gate": b_gate,
        "mlstm_norm_g": 1.0 + nrm(ks[16], (DEPTH, M_HEADS, V_DIM), 0.1),
        "conv_w": nrm(ks[17], (DEPTH, CONV_WIDTH, D_CONV), CONV_WIDTH ** -0.5),
        "conv_b": nrm(ks[18], (DEPTH, D_CONV), 0.02),
        "conv_ln_g": 1.0 + nrm(ks[19], (DEPTH, D_CONV), 0.1),
        "conv_ln_b": nrm(ks[20], (DEPTH, D_CONV), 0.02),
        "w_out": nrm(ks[21], (DEPTH, D_MIX, D), D_MIX ** -0.5),
        "b_out": nrm(ks[22], (DEPTH, D), 0.02),
        "w_router": nrm(ks[23], (DEPTH, D, N_EXPERTS), D ** -0.5),
        "b_router": nrm(ks[24], (DEPTH, N_EXPERTS), 0.01),
        "we_gate": nrm(ks[25], (DEPTH, N_EXPERTS, D, D_EXPERT), D ** -0.5),
        "we_up": nrm(ks[26], (DEPTH, N_EXPERTS, D, D_EXPERT), D ** -0.5),
        "we_down": nrm(ks[27], (DEPTH, N_EXPERTS, D_EXPERT, D), D_EXPERT ** -0.5),
        "ws_gate": nrm(ks[28], (DEPTH, D, D_SHARED), D ** -0.5),
        "ws_up": nrm(ks[29], (DEPTH, D, D_SHARED), D ** -0.5),
        "ws_down": nrm(ks[30], (DEPTH, D_SHARED, D), D_SHARED ** -0.5),
    }


def reference(x, c, ctx, c_ctx, w_ada, b_ada, g_pre_mix, g_post_mix, g_pre_ffn, g_post_ffn,
              w_in, b_in, b_gate, mlstm_norm_g, conv_w, conv_b, conv_ln_g, conv_ln_b, w_out, b_out,
              w_router, b_router, we_gate, we_up, we_down, ws_gate, ws_up, ws_down):
    for l in range(DEPTH):
        ctx_out = l < DEPTH - 1
        mx = [t[:, None, :] for t in jnp.split(jax.nn.silu(c) @ w_ada[l] + b_ada[l], 6, axis=-1)]
        mc = jnp.split(jax.nn.silu(c_ctx) @ w_ada[l] + b_ada[l], 6, axis=-1)
        hx = _modulate(_rms_norm(x, g_pre_mix[l]), mx[0], mx[1])
        hc = _modulate(_rms_norm(ctx, g_pre_mix[l]), mc[0], mc[1])
        yx, yc = _token_mixer(hx, hc, w_in[l], b_in[l], b_gate[l], mlstm_norm_g[l], conv_w[l], conv_b[l],
                              conv_ln_g[l], conv_ln_b[l], w_out[l], b_out[l], ctx_out)
        x = x + mx[2] * _rms_norm(yx, g_post_mix[l])
        hx = _modulate(_rms_norm(x, g_pre_ffn[l]), mx[3], mx[4])
        x = x + mx[5] * _rms_norm(_moe_ffn(hx, w_router[l], b_router[l], we_gate[l], we_up[l], we_down[l],
                                           ws_gate[l], ws_up[l], ws_down[l]), g_post_ffn[l])
        if ctx_out:
            ctx = ctx + mc[2] * _rms_norm(yc, g_post_mix[l])
            hc = _modulate(_rms_norm(ctx, g_pre_ffn[l]), mc[3], mc[4])
            ctx = ctx + mc[5] * _rms_norm(_moe_ffn(hc, w_router[l], b_router[l], we_gate[l], we_up[l], we_down[l],
                                                 ws_gate[l], ws_up[l], ws_down[l]), g_post_ffn[l])
    return x
```

```python
import os
import numpy as np
from contextlib import ExitStack
import concourse.bass as bass
import concourse.mybir as mybir
from concourse.bass_utils import run_bass_kernel_spmd

F32 = mybir.dt.float32
BF16 = mybir.dt.bfloat16
AF = mybir.ActivationFunctionType
ALU = mybir.AluOpType
AX = mybir.AxisListType

D = 1024
NL = 4096
NCH = NL // 128
CTX = 256
NE = 64
EPS = 1e-6
DIN = 2576
ARENA_WORDS = 53000


class Op:
    __slots__ = ("eng", "fn", "reads", "writes", "dma", "deps", "milestone", "mval", "msem", "pos", "dval", "waits")

    def __init__(self, eng, fn, reads, writes, dma):
        self.eng = eng
        self.fn = fn
        self.reads = tuple(reads)
        self.writes = tuple(writes)
        self.dma = dma
        self.milestone = False
        self.mval = 0
        self.msem = 0
        self.dval = 0
        self.waits = []


class _Recorder:
    def __getattr__(self, name):
        def f(*args, **kw):
            self.last = (name, args, kw)
            return self
        return f


class Sched:
    ENGS = ("pe", "act", "dve", "pool", "sp")
    CAP = 16000

    def __init__(self, nc):
        self.nc = nc
        self.ops = []
        self._barrier = False
        self.ps_touch = {}

    def add(self, eng, fn, r=(), w=(), dma=None):
        if self._barrier and fn is not None:
            r = tuple(r) + ("__barrier__",)
        op = Op(eng, fn, r, w, dma)
        self.ops.append(op)
        for k in op.reads + op.writes:
            if isinstance(k, str) and k.startswith("ps") and len(k) == 3:
                self.ps_touch[k] = len(self.ops)
        return op

    def pe(self, fn, r=(), w=()):
        return self.add("pe", fn, r, w)

    def act(self, fn, r=(), w=()):
        return self.add("act", fn, r, w)

    def dve(self, fn, r=(), w=()):
        return self.add("dve", fn, r, w)

    def pool(self, fn, r=(), w=()):
        return self.add("pool", fn, r, w)

    def dma(self, q, fn, key, r=(), w=()):
        return self.add(q, fn, r, w, dma=key)

    def barrier(self):
        self.add("sp", None, r=(), w=("__barrier__",), dma=None)
        self._barrier = True

    reorder = os.environ.get("NOREORDER") is None

    @staticmethod
    def _cost(op):
        if op.fn is None:
            return 50.0, 50.0
        rec = _Recorder()
        op.fn(rec)
        name, args, kw = rec.last

        def fsz(ap):
            n = 1
            for d in ap.shape[1:]:
                n *= d
            return n
        if name == "dma_start":
            o = kw.get("out")
            nbytes = fsz(o) * o.shape[0] * 4
            issue = 1200.0 if op.eng == "pool" else 120.0
            return issue, issue + 2200.0 + nbytes / 150.0
        if name == "matmul":
            n = fsz(kw["rhs"])
            c = max(n, 48) * 0.55 + 25
            if kw["rhs"].dtype == F32:
                c *= 4
            return c, c + 120
        if name == "transpose":
            c = 100.0 if kw["in_"].dtype != F32 else 160.0
            return c, c + 120
        if name in ("tensor_reduce", "max"):
            n = fsz(kw["in_"])
        elif name == "memset":
            n = fsz(args[0] if args else kw["ap"])
        else:
            o = kw.get("out", args[0] if args else None)
            n = fsz(o)
        if op.eng == "act":
            c = 200 + 0.85 * n + (95 if kw.get("accum_out") is not None else 0)
        elif op.eng == "dve":
            psum = any(getattr(kw.get(k), "space", None) is not None and "PSUM" in str(kw.get(k).space).upper() for k in ("in_", "in0", "in1"))
            c = 65 + 1.05 * n + (65 if psum else 0)
        else:
            c = 150 + 1.8 * n
        return c, c + 60

    def _list_schedule(self, ops, WIN=10 ** 9):
        n = len(ops)
        costs = [self._cost(op) for op in ops]
        uid = [0] * n
        units = []
        open_u = None
        for i, op in enumerate(ops):
            if op.eng == "pe" and op.fn is not None:
                rec = _Recorder()
                op.fn(rec)
                name, args, kw = rec.last
                start = kw.get("start", True) if name == "matmul" else True
                stop = kw.get("stop", True) if name == "matmul" else True
                if start or open_u is None:
                    units.append([i])
                    open_u = len(units) - 1
                else:
                    units[open_u].append(i)
                uid[i] = open_u
                if stop:
                    open_u = None
            else:
                units.append([i])
                uid[i] = len(units) - 1
        nu = len(units)
        udeps = [set() for _ in range(nu)]
        for i, op in enumerate(ops):
            u = uid[i]
            for j in op.deps:
                if uid[j] != u:
                    udeps[u].add(uid[j])
        succ = [[] for _ in range(nu)]
        indeg = [len(d) for d in udeps]
        for u, d in enumerate(udeps):
            for v in d:
                succ[v].append(u)
        ueng = [ops[m[0]].eng for m in units]
        uocc = [sum(costs[i][0] for i in m) for m in units]
        ulat = [uocc[u] - costs[m[-1]][0] + costs[m[-1]][1] for u, m in enumerate(units)]
        rt = [0.0] * nu
        fin = [0.0] * nu
        free = {e: 0.0 for e in self.ENGS}
        ready = {e: [] for e in self.ENGS}
        tail = [0.0] * nu
        if os.environ.get("SCHED_PRIO", "cp") == "cp":
            for u in range(nu - 1, -1, -1):
                t = 0.0
                for v in succ[u]:
                    if tail[v] > t:
                        t = tail[v]
                tail[u] = t + ulat[u]
        for u in range(nu):
            if indeg[u] == 0:
                ready[ueng[u]].append(u)
        order = []
        done = 0
        kb_unit = max([uid[i] for i, op in enumerate(ops) if op.fn is None] + [-1])
        while done < nu:
            best = None
            for e in self.ENGS:
                lst = ready[e]
                if not lst:
                    continue
                f = free[e]
                bkey = None
                lo = min(lst)
                for u in lst:
                    if u > lo + WIN:
                        continue
                    stt = rt[u] if rt[u] > f else f
                    key = (stt, -tail[u], u)
                    if bkey is None or key < bkey:
                        bkey = key
                if best is None or bkey < best[0]:
                    best = (bkey, e)
            (stt, _, u), e = best
            ready[e].remove(u)
            free[e] = stt + uocc[u]
            fin[u] = stt + ulat[u]
            order.extend(units[u])
            done += 1
            for s_ in succ[u]:
                indeg[s_] -= 1
                if fin[u] > rt[s_]:
                    rt[s_] = fin[u]
                if indeg[s_] == 0:
                    ready[ueng[s_]].append(s_)
        self.est_ns = max(fin) if fin else 0.0
        self.barrier_ns = [fin[uid[i]] for i, op in enumerate(ops) if op.fn is None]
        busy = {e: 0.0 for e in self.ENGS}
        for i, op in enumerate(ops):
            busy[op.eng] += costs[i][0]
        self.busy_ns = busy
        inv = [0] * n
        for newi, oldi in enumerate(order):
            inv[oldi] = newi
        new_ops = [ops[i] for i in order]
        for op in new_ops:
            op.deps = {inv[j]: raw for j, raw in op.deps.items()}
        return new_ops

    def emit(self, stack):
        nc = self.nc
        ops = self.ops
        allkeys = set()
        for op in ops:
            if op.fn is None:
                op.reads = tuple(allkeys)
                op.writes = tuple(allkeys | {"__barrier__"})
                continue
            allkeys.update(op.reads)
            allkeys.update(op.writes)
        cnt = {e: 0 for e in self.ENGS}
        for op in ops:
            op.pos = cnt[op.eng]
            cnt[op.eng] += 1
        last_w = {}
        readers = {}
        for i, op in enumerate(ops):
            deps = {}
            for k in op.reads:
                j = last_w.get(k)
                if j is not None:
                    deps[j] = True
            for k in op.writes:
                j = last_w.get(k)
                if j is not None and j not in deps:
                    deps[j] = False
                for j in readers.get(k, ()):
                    if j not in deps:
                        deps[j] = False
            deps.pop(i, None)
            op.deps = deps
            for k in op.reads:
                readers.setdefault(k, []).append(i)
            for k in op.writes:
                last_w[k] = i
                readers[k] = []
        if self.reorder:
            mode = os.environ.get("REORDER_MODE", "023")
            mode = {"moe": "3", "all": "0123"}.get(mode, mode)
            segwin = {int(kv.split(":")[0]): int(kv.split(":")[1]) for kv in os.environ.get("SEGWIN", "").split(",") if kv}
            bidx = [i for i, op in enumerate(ops) if op.fn is None]
            bounds = [-1] + bidx + [len(ops)]
            segs = []
            for si_ in range(len(bounds) - 1):
                lo_, hi_ = bounds[si_] + 1, bounds[si_ + 1]
                seg = ops[lo_:hi_]
                if str(si_) in mode and seg:
                    for op in seg:
                        op.deps = {j - lo_: raw for j, raw in op.deps.items() if j >= lo_}
                    seg = self._list_schedule(seg, segwin.get(si_, 10 ** 9))
                segs.extend(seg)
                if hi_ < len(ops):
                    segs.append(ops[hi_])
            ops = segs
            last_w = {}
            readers = {}
            for i, op in enumerate(ops):
                deps = {}
                for k in op.reads:
                    j = last_w.get(k)
                    if j is not None:
                        deps[j] = True
                for k in op.writes:
                    j = last_w.get(k)
                    if j is not None and j not in deps:
                        deps[j] = False
                    for j in readers.get(k, ()):
                        if j not in deps:
                            deps[j] = False
                deps.pop(i, None)
                op.deps = deps
                for k in op.reads:
                    readers.setdefault(k, []).append(i)
                for k in op.writes:
                    last_w[k] = i
                    readers[k] = []
            self.ops = ops
            cnt = {e: 0 for e in self.ENGS}
            for op in ops:
                op.pos = cnt[op.eng]
                cnt[op.eng] += 1
        dma_tot = {}
        for op in ops:
            if op.dma is not None:
                dma_tot[op.dma] = dma_tot.get(op.dma, 0) + 16
                op.dval = dma_tot[op.dma]
        for op in ops:
            if op.dma is not None and op.dma.startswith("G:"):
                op.dval = dma_tot[op.dma]
        need = []
        SAME_ALL = int(os.environ.get("SAME_ALL", 0))
        for i, op in enumerate(ops):
            lst = []
            for j, raw in op.deps.items():
                P = ops[j]
                if P.dma is not None:
                    if not (op.dma == P.dma and P.dma.startswith("G:")):
                        lst.append(j)
                elif P.eng == op.eng:
                    if op.dma is not None or P.fn is None or op.fn is None:
                        lst.append(j)
                    elif raw and (op.pos - P.pos) <= 2 and op.eng != "pe":
                        lst.append(j)
                    elif SAME_ALL and op.eng != "pe" and (op.pos - P.pos) <= SAME_ALL:
                        lst.append(j)
                else:
                    lst.append(j)
            need.append(lst)
            for j in lst:
                if ops[j].dma is None:
                    ops[j].milestone = True
        last_on = {}
        for i, op in enumerate(ops):
            if op.dma is None:
                last_on[op.eng] = i
        for e, i in last_on.items():
            ops[i].milestone = True
        mcnt = {e: 0 for e in self.ENGS}
        for op in ops:
            if op.dma is None and op.milestone:
                op.msem = mcnt[op.eng] // self.CAP
                op.mval = mcnt[op.eng] % self.CAP + 1
                mcnt[op.eng] += 1
        esem = {}
        for e in self.ENGS:
            n = (mcnt[e] + self.CAP - 1) // self.CAP
            esem[e] = [stack.enter_context(nc.semaphore("sem_%s%d" % (e, q))) for q in range(max(n, 1))]
        dsem = {k: stack.enter_context(nc.semaphore("dsem_%d" % n)) for n, k in enumerate(dma_tot)}
        self.n_sems = sum(len(v) for v in esem.values()) + len(dsem)
        seen = {e: {} for e in self.ENGS}
        streams = {e: [] for e in self.ENGS}
        for i, op in enumerate(ops):
            waits = {}
            for j in need[i]:
                P = ops[j]
                if P.dma is not None:
                    s, v = dsem[P.dma], P.dval
                else:
                    s, v = esem[P.eng][P.msem], P.mval
                sid = id(s)
                if seen[op.eng].get(sid, 0) >= v:
                    continue
                if sid not in waits or waits[sid][1] < v:
                    waits[sid] = (s, v)
            for sid, (s, v) in waits.items():
                seen[op.eng][sid] = v
            op.waits = list(waits.values())
            streams[op.eng].append(op)
        final_waits = [(dsem[k], v) for k, v in dma_tot.items()]
        for e, i in last_on.items():
            if e != "sp":
                final_waits.append((esem[e][ops[i].msem], ops[i].mval))
        semval = {}
        ptr = {e: 0 for e in self.ENGS}
        progress = True
        while progress:
            progress = False
            for en in self.ENGS:
                st_ = streams[en]
                while ptr[en] < len(st_):
                    op = st_[ptr[en]]
                    if any(semval.get(id(s_), 0) < v_ for s_, v_ in op.waits):
                        break
                    if op.dma is not None:
                        sm = dsem[op.dma]
                        semval[id(sm)] = semval.get(id(sm), 0) + 16
                    elif op.milestone:
                        sm = esem[en][op.msem]
                        semval[id(sm)] = semval.get(id(sm), 0) + 1
                    ptr[en] += 1
                    progress = True
        stuck = {en: ptr[en] for en in self.ENGS if ptr[en] < len(streams[en])}
        if stuck:
            for en, p in stuck.items():
                op = streams[en][p]
                print("DEADLOCK: engine", en, "op#", p, "dma" if op.dma else "", op.dma, "reads", op.reads[:6], "writes", op.writes[:6],
                      "waits", [(s_.name if hasattr(s_, "name") else str(s_), v_, semval.get(id(s_), 0)) for s_, v_ in op.waits])
            raise RuntimeError("scheduler deadlock")
        block = stack.enter_context(nc.Block())

        def run(e, name):
            for op in streams[name]:
                for s, v in op.waits:
                    e.wait_ge(s, v)
                if op.fn is None:
                    ins = e.nop()
                else:
                    ins = op.fn(e)
                if op.dma is not None:
                    ins.then_inc(dsem[op.dma], 16)
                elif op.milestone:
                    ins.then_inc(esem[name][op.msem], 1)
            if name == "sp":
                for s, v in final_waits:
                    e.wait_ge(s, v)

        @block.tensor
        def _(e):
            run(e, "pe")

        @block.scalar
        def _(e):
            run(e, "act")

        @block.vector
        def _(e):
            run(e, "dve")

        @block.gpsimd
        def _(e):
            run(e, "pool")

        @block.sync
        def _(e):
            run(e, "sp")

        self.stats = {e: len(streams[e]) for e in self.ENGS}


class Buf:
    def __init__(self, h, base, pitch, shape, key):
        self.h = h
        self.base = base
        self.pitch = pitch
        self.shape = list(shape)
        self.key = key
        st = [1] * (len(shape) - 1)
        for i in range(len(shape) - 3, -1, -1):
            st[i] = st[i + 1] * shape[i + 2]
        self.strides = st

    def raw(self, p0, npart, off, dims):
        return bass.AP(self.h, p0 * self.pitch + self.base + off, [[self.pitch, npart]] + [list(d) for d in dims])

    def __getitem__(self, idx):
        if not isinstance(idx, tuple):
            idx = (idx,)
        idx = list(idx) + [slice(None)] * (len(self.shape) - len(idx))
        ps = idx[0]
        if isinstance(ps, int):
            p0, npart = ps, 1
        else:
            p0 = ps.start or 0
            npart = (ps.stop if ps.stop is not None else self.shape[0]) - p0
        off = 0
        dims = []
        for d, ix in enumerate(idx[1:]):
            if isinstance(ix, int):
                off += ix * self.strides[d]
            else:
                a = ix.start or 0
                b = ix.stop if ix.stop is not None else self.shape[d + 1]
                off += a * self.strides[d]
                dims.append([self.strides[d], b - a])
        if not dims:
            dims = [[1, 1]]
        merged = [dims[0]]
        for s, c in dims[1:]:
            ps_, pc = merged[-1]
            if ps_ == s * c:
                merged[-1] = [s, pc * c]
            else:
                merged.append([s, c])
        return self.raw(p0, npart, off, merged)


class Arena:
    def __init__(self, hf, hb, words):
        self.hf = hf
        self.hb = hb
        self.words = words
        self.top = 0
        self.n = 0

    def alloc(self, shape, dt, key=None):
        n = 1
        for s in shape[1:]:
            n *= s
        w = n if dt == F32 else (n + 1) // 2
        w = (w + 7) // 8 * 8
        off = self.top
        self.top += w
        assert self.top <= self.words, "arena overflow %d" % self.top
        self.n += 1
        key = key or ("t%d" % self.n)
        if dt == F32:
            return Buf(self.hf, off, self.words, shape, key)
        return Buf(self.hb, off * 2, self.words * 2, shape, key)

    def mark(self):
        return self.top

    def release(self, m):
        self.top = m


def build(debug=False, phases=3):
    nc = bass.Bass("TRN2", target_bir_lowering=False)

    def din(name, shape):
        return nc.dram_tensor(name, shape, F32, kind="ExternalInput").ap()

    xL = din("xL", [NL, D])
    xO = din("xO", [NL, D])
    cx = din("cx", [CTX, D])
    c_t = din("c_t", [128, 16])
    w_ada = din("w_ada", [D, 6 * D])
    b_ada = din("b_ada", [1, 6 * D])
    gvec = din("gvec", [4, D])
    w_in = din("w_in", [D, DIN])
    b_in = din("b_in", [1, DIN])
    b_gate = din("b_gate", [1, 16])
    ng = din("ng", [1, 512])
    convp = din("convp", [128, 4 * 34])
    w_out = din("w_out", [D, D])
    b_out = din("b_out", [1, D])
    w_r = din("w_r", [D, NE])
    b_r = din("b_r", [1, NE])
    we_gu = din("we_gu", [NE + 1, D, 512])
    we_d = din("we_d", [NE + 1, 256, D])
    consts = din("consts", [128, 5 * 128])
    sel = din("sel", [2, 256])
    out = nc.dram_tensor("out", [NL, D], F32, kind="ExternalOutput").ap()
    x1s = nc.dram_tensor("x1s", [NL, D], F32, kind="Internal").ap()
    hts = nc.dram_tensor("hts", [128, 8 * NL], BF16, kind="Internal").ap()
    tmpbs = nc.dram_tensor("tmpbs", [NCH, 64, 4 * 130], BF16, kind="Internal").ap()
    dbg_out = {}

    S = Sched(nc)
    with ExitStack() as st:
        hf = st.enter_context(nc.sbuf_tensor("arena", [128, ARENA_WORDS], F32))
        hb = hf.bitcast(BF16)
        AR = Arena(hf, hb, ARENA_WORDS)
        psf = [st.enter_context(nc.psum_tensor("ps%d" % i, [128, 512], F32)) for i in range(8)]
        psb = [p.bitcast(BF16) for p in psf]
        PF = [Buf(psf[i], 0, 512, [128, 512], "ps%d" % i) for i in range(8)]
        PB = [Buf(psb[i], 0, 1024, [128, 1024], "ps%d" % i) for i in range(8)]
        pctr = [0]

        def bank():
            if os.environ.get("BANK_RR"):
                i = pctr[0] % 8
                pctr[0] += 1
                return i
            i = min(range(8), key=lambda b: S.ps_touch.get("ps%d" % b, -1))
            S.ps_touch["ps%d" % i] = len(S.ops) + 0.5
            return i

        def dbg(name, buf, shape2d, apfn):
            if not debug:
                return
            t = nc.dram_tensor("dbg_" + name, list(shape2d), F32, kind="ExternalOutput").ap()
            dbg_out[name] = shape2d
            S.dma("pool", lambda e: e.dma_start(out=t, in_=apfn()), "dbg_" + name, r=[buf.key])

        IDB = AR.alloc([128, 128], BF16, "IDB")
        ONESB = AR.alloc([128, 128], BF16, "ONESB")
        CF = AR.alloc([128, 5, 128], F32, "CF")
        WTS = AR.alloc([128, NCH, NE + 1], F32, "WTS")
        A4x = AR.alloc([128, D], F32, "A4x")
        persist_mark = AR.mark()

        S.dma("sp", lambda e: e.dma_start(out=CF[:], in_=consts), "G:c0", w=["CF"])
        S.dma("pool", lambda e: e.dma_start(out=IDB[:], in_=consts[:, 0:128]), "G:c1", w=["IDB"])
        S.dma("pool", lambda e: e.dma_start(out=ONESB[:], in_=consts[:, 384:512]), "G:c1", w=["ONESB"])
        IDF = lambda: CF[:, 0, :]
        MA = lambda: CF[:, 1, :]
        MB = lambda: CF[:, 2, :]
        ONESF = lambda: CF[:, 3, :]
        ONES512 = lambda: CF[:, 4, :]
        S.pool(lambda e: e.memset(WTS[:], 1.0), w=["WTS"])

        WIN = AR.alloc([128, 8, DIN], BF16, "WIN")
        WOUT = AR.alloc([128, 8, D], BF16, "WOUT")
        BINR = AR.alloc([1, DIN], BF16, "BINR")
        BGR = AR.alloc([1, 16], BF16, "BGR")
        BOUTR = AR.alloc([1, D], BF16, "BOUTR")
        ONER = AR.alloc([1, 512], BF16, "ONER")
        A1x = AR.alloc([128, D], BF16, "A1x")
        S1x = AR.alloc([128, D], BF16, "S1x")
        A1c = AR.alloc([128, D], BF16, "A1c")
        S1c = AR.alloc([128, D], BF16, "S1c")
        A2x = AR.alloc([128, D], F32, "A2x")
        A3x = AR.alloc([128, D], F32, "A3x")
        S3x = AR.alloc([128, D], F32, "S3x")
        NGB = AR.alloc([128, 512], F32, "NGB")
        CONVP = AR.alloc([128, 4, 34], F32, "CONVP")
        WRF = AR.alloc([128, 8, NE], F32, "WRF")
        BRB = AR.alloc([128, NE], F32, "BRB")

        S.dma("pool", lambda e: e.dma_start(out=WIN[:], in_=w_in.rearrange("(k p) n -> p k n", p=128)), "G:wl", w=["WIN"])
        S.dma("pool", lambda e: e.dma_start(out=WOUT[:], in_=w_out.rearrange("(k p) n -> p k n", p=128)), "G:wl", w=["WOUT"])
        S.dma("pool", lambda e: e.dma_start(out=BINR[:], in_=b_in), "G:wl", w=["BINR"])
        S.dma("pool", lambda e: e.dma_start(out=BGR[:], in_=b_gate), "G:wl", w=["BGR"])
        S.dma("pool", lambda e: e.dma_start(out=BOUTR[:], in_=b_out), "G:wl", w=["BOUTR"])
        S.dma("sp", lambda e: e.dma_start(out=NGB[:], in_=ng.partition_broadcast(128)), "G:c0", w=["NGB"])
        S.dma("sp", lambda e: e.dma_start(out=BRB[:], in_=b_r.partition_broadcast(128)), "G:c0", w=["BRB"])
        S.dma("sp", lambda e: e.dma_start(out=CONVP[:], in_=convp), "G:c0", w=["CONVP"])
        S.dma("sp", lambda e: e.dma_start(out=WRF[:], in_=w_r.rearrange("(k p) n -> p k n", p=128)), "G:c0", w=["WRF"])
        S.pool(lambda e: e.memset(ONER[:], 1.0), w=["ONER"])

        mA = AR.mark()
        CT = AR.alloc([128, 16], F32)
        SC = AR.alloc([128, 16], BF16)
        WAD = AR.alloc([128, 8, 768], BF16)
        MOD = AR.alloc([2, 6 * D], F32)
        BADc = [AR.alloc([2, 512], F32) for _ in range(2)]
        GV = AR.alloc([2, 4, D], F32)
        SEL = AR.alloc([2, 256], F32)
        S.dma("sp", lambda e: e.dma_start(out=CT[:], in_=c_t), "G:a0", w=[CT.key])
        S.dma("sp", lambda e: e.dma_start(out=SEL[:], in_=sel), "G:a0", w=[SEL.key])
        for gi in range(4):
            S.dma("sp", lambda e, gi=gi: e.dma_start(out=GV[:, gi, :], in_=gvec[gi:gi + 1, :].partition_broadcast(2)), "G:a0", w=[GV.key])
        S.act(lambda e: e.activation(out=SC[:], in_=CT[:], func=AF.Silu), r=[CT.key], w=[SC.key])
        for piece in range(8):
            S.dma("pool", lambda e, piece=piece: e.dma_start(
                out=WAD[:], in_=w_ada[:, piece * 768:(piece + 1) * 768].rearrange("(k p) n -> p k n", p=128)),
                "wad", w=[WAD.key])
            for nn in range(2):
                bk = bank()
                c0 = piece * 768 + nn * 384
                bd = BADc[(piece * 2 + nn) % 2]
                S.dma("sp", lambda e, bd=bd, c0=c0: e.dma_start(out=bd[:, 0:384], in_=b_ada[:, c0:c0 + 384].partition_broadcast(2)),
                      "bad%d" % ((piece * 2 + nn) % 2), w=[bd.key])
                for k in range(8):
                    S.pe(lambda e, bk=bk, k=k, nn=nn: e.matmul(PF[bk][0:2, 0:384], lhsT=SC.raw(0, 128, k, [[8, 2]]),
                                                               rhs=WAD[:, k, nn * 384:(nn + 1) * 384], start=(k == 0), stop=(k == 7)),
                         r=[SC.key, WAD.key], w=[PF[bk].key])
                S.dve(lambda e, bk=bk, c0=c0, bd=bd: e.tensor_tensor(out=MOD[:, c0:c0 + 384], in0=PF[bk][0:2, 0:384], in1=bd[:, 0:384], op=ALU.add),
                      r=[PF[bk].key, bd.key], w=[MOD.key])
        dbg("MOD", MOD, [2, 6 * D], lambda: MOD[:])
        dbg("SC", SC, [128, 16], lambda: SC[:])
        dbg("GV", GV, [2, 4 * D], lambda: GV[:])
        S.dve(lambda e: e.scalar_tensor_tensor(out=MOD[:, D:2 * D], in0=MOD[:, D:2 * D], scalar=1.0, in1=GV[:, 0, :], op0=ALU.add, op1=ALU.mult),
              r=[MOD.key, GV.key], w=[MOD.key])
        S.dve(lambda e: e.tensor_tensor(out=MOD[:, 2 * D:3 * D], in0=MOD[:, 2 * D:3 * D], in1=GV[:, 1, :], op=ALU.mult), r=[MOD.key, GV.key], w=[MOD.key])
        S.dve(lambda e: e.scalar_tensor_tensor(out=MOD[:, 4 * D:5 * D], in0=MOD[:, 4 * D:5 * D], scalar=1.0, in1=GV[:, 2, :], op0=ALU.add, op1=ALU.mult),
              r=[MOD.key, GV.key], w=[MOD.key])
        S.dve(lambda e: e.tensor_tensor(out=MOD[:, 5 * D:6 * D], in0=MOD[:, 5 * D:6 * D], in1=GV[:, 3, :], op=ALU.mult), r=[MOD.key, GV.key], w=[MOD.key])
        for (dst, vi, row) in ((A1x, 1, 0), (S1x, 0, 0), (A1c, 1, 1), (S1c, 0, 1), (A2x, 2, 0), (A3x, 4, 0), (S3x, 3, 0), (A4x, 5, 0)):
            for hh in range(2):
                bk = bank()
                S.pe(lambda e, bk=bk, vi=vi, row=row, hh=hh: e.matmul(PF[bk][:, :], lhsT=SEL[:, row * 128:(row + 1) * 128],
                                                                      rhs=MOD[:, vi * D + hh * 512:vi * D + (hh + 1) * 512], start=True, stop=True),
                     r=[SEL.key, MOD.key], w=[PF[bk].key])
                S.act(lambda e, bk=bk, dst=dst, hh=hh: e.activation(out=dst[:, hh * 512:(hh + 1) * 512], in_=PF[bk][:, :], func=AF.Copy),
                      r=[PF[bk].key], w=[dst.key])
        dbg("A1x", A1x, [128, D], lambda: A1x[:])
        dbg("S3x", S3x, [128, D], lambda: S3x[:])
        dbg("A1c", A1c, [128, D], lambda: A1c[:])
        AR.release(mA)
        S.barrier()
        DIAG = AR.alloc([128, 124, 128], BF16, "DIAG")
        WBs = AR.alloc([128, NCH, 4], F32, "WBs")
        FLBs = AR.alloc([128, NCH, 4], F32, "FLBs")
        CST = [AR.alloc([64, 4, 130], F32, "CST%d" % d) for d in range(2)]
        MST = [AR.alloc([128, 4], F32, "MST%d" % d) for d in range(2)]
        for d in range(2):
            S.pool(lambda e, d=d: e.memset(CST[d][:], 0.0), w=[CST[d].key])
            S.pool(lambda e, d=d: e.memset(MST[d][:], 0.0), w=[MST[d].key])
        for cc in range(4):
            for j in range(31):
                S.pool(lambda e, cc=cc, j=j: e.tensor_scalar(out=DIAG[:, cc * 31 + j, :], in0=CF[:, 0, :],
                                                          scalar1=CONVP[:, cc, j:j + 1], scalar2=None, op0=ALU.mult),
                       r=["CF", "CONVP"], w=["DIAG"])

        NB = 2

        def wtiles(shape, dt, n=NB):
            return [AR.alloc(shape, dt) for _ in range(n)]

        XT = wtiles([128, D], F32, 2)
        JUNK = AR.alloc([128, D], BF16)
        SSQ = wtiles([128, 4], F32)
        TMPF = wtiles([128, D], F32)
        HXB = wtiles([128, D], BF16, 1) * 2
        HXT = wtiles([128, 8, 128], BF16)

        def rms_rstd(i, src_ap_fn, srckeys, width, col):
            q = SSQ[i % NB]
            S.act(lambda e: e.activation(out=JUNK[:, 0:width], in_=src_ap_fn(), func=AF.Square, accum_out=q[:, col:col + 1]),
                  r=srckeys, w=[JUNK.key, q.key])

        def finish_rstd(i, col, n, scale):
            q = SSQ[i % NB]
            S.act(lambda e: e.activation(out=q[:, col:col + n], in_=q[:, col:col + n], func=AF.Sqrt, scale=scale, bias=EPS), r=[q.key], w=[q.key])
            S.dve(lambda e: e.reciprocal(out=q[:, col:col + n], in_=q[:, col:col + n]), r=[q.key], w=[q.key])

        def norm_to_hxT(i, xt, Ab, Sb):
            q = SSQ[i % NB]
            tf = TMPF[i % NB]
            hx = HXB[i % NB]
            hT = HXT[i % NB]
            rms_rstd(i, lambda: xt[:], [xt.key], D, 0)
            finish_rstd(i, 0, 1, 1.0 / D)
            S.dve(lambda e: e.scalar_tensor_tensor(out=tf[:], in0=xt[:], scalar=q[:, 0:1], in1=Ab[:], op0=ALU.mult, op1=ALU.mult),
                  r=[xt.key, q.key, Ab.key], w=[tf.key])
            S.pool(lambda e: e.tensor_tensor(out=hx[:], in0=tf[:], in1=Sb[:], op=ALU.add), r=[tf.key, Sb.key], w=[hx.key])
            bk = bank()
            for k in range(8):
                S.pe(lambda e, k=k, bk=bk: e.transpose(out=PB[bk][:, k * 128:(k + 1) * 128], in_=hx[:, k * 128:(k + 1) * 128], identity=IDB[:]),
                     r=[hx.key, "IDB"], w=[PB[bk].key])
            S.act(lambda e, bk=bk: e.activation(out=hT[:], in_=PB[bk][:, :], func=AF.Copy), r=[PB[bk].key], w=[hT.key])
            return hT

        def proj_tok(hT, bk, c0, n, gate_bias=False):
            for k in range(8):
                S.pe(lambda e, k=k: e.matmul(PF[bk][:, 0:n], lhsT=hT[:, k, :], rhs=WIN[:, k, c0:c0 + n], start=(k == 0), stop=False),
                     r=[hT.key, "WIN"], w=[PF[bk].key])
            S.pe(lambda e: e.matmul(PF[bk][:, 0:n], lhsT=ONER[0:1, 0:128], rhs=BINR[0:1, c0:c0 + n], start=False, stop=not gate_bias),
                 r=["ONER", "BINR"], w=[PF[bk].key])
            if gate_bias:
                S.pe(lambda e: e.matmul(PF[bk][:, n - 16:n], lhsT=ONER[0:1, 0:128], rhs=BGR[0:1, 0:16], start=False, stop=True),
                     r=["ONER", "BGR"], w=[PF[bk].key])

        def proj_feat(hT, bk, off, c0, m, npart=None):
            for k in range(8):
                S.pe(lambda e, k=k: e.matmul(PF[bk][0:m, off:off + 128], lhsT=WIN[:, k, c0:c0 + m], rhs=hT[:, k, :], start=(k == 0), stop=False),
                     r=[hT.key, "WIN"], w=[PF[bk].key])
            S.pe(lambda e: e.matmul(PF[bk][0:m, off:off + 128], lhsT=BINR[0:1, c0:c0 + m], rhs=ONER[0:1, 0:128], start=False, stop=True),
                 r=["ONER", "BINR"], w=[PF[bk].key])

        CQ, CK, CG, CV, CO, CGA, CGB = 0, 256, 512, 528, 1040, 1552, 2064

        GSB = wtiles([128, 16], F32)
        SP = wtiles([128, 4], F32)
        DD = wtiles([128, 4], F32)
        NBT = wtiles([128, 4], F32)
        RR = wtiles([128, 4, 128], BF16)
        MXB = wtiles([128, 4], F32)
        BTOT = wtiles([128, 4], F32)
        WLOC = wtiles([128, 4], F32)
        MM = wtiles([128, 4], F32)
        DM = wtiles([128, 2, 4], F32)
        AE = wtiles([128, 2, 4], F32)
        WS = wtiles([128, 4], F32)
        FL = wtiles([128, 4], F32)
        KSB = wtiles([128, 256], BF16)
        WV = wtiles([128, 4, 130], BF16)
        TMPS = wtiles([64, 4, 130], F32, 1) * 2
        TMPSB = wtiles([64, 4, 130], BF16)
        LE = wtiles([64, 4, 130], F32, 1) * 2

        def gate_pack(i, bkg, d, kcol0=0, gcol0=256):
            j = i % NB
            g, sp, dd, nbt, rr, mxb, btot, wloc = GSB[j], SP[j], DD[j], NBT[j], RR[j], MXB[j], BTOT[j], WLOC[j]
            li0, lf0 = (0, 4) if d == 0 else (8, 12)
            S.dve(lambda e: e.tensor_copy(out=g[:], in_=PF[bkg][:, gcol0:gcol0 + 16]), r=[PF[bkg].key], w=[g.key])
            S.act(lambda e: e.activation(out=sp[:], in_=g[:, lf0:lf0 + 4], func=AF.Exp, scale=-1.0), r=[g.key], w=[sp.key])
            S.act(lambda e: e.activation(out=sp[:], in_=sp[:], func=AF.Ln, bias=1.0), r=[sp.key], w=[sp.key])
            bk = bank()
            msk = MA if d == 0 else MB
            S.pe(lambda e: e.matmul(PF[bk][:, 0:4], lhsT=msk(), rhs=sp[:], start=True, stop=True), r=["CF", sp.key], w=[PF[bk].key])
            S.pe(lambda e: e.matmul(PF[bk][:, 4:8], lhsT=ONESF(), rhs=sp[:], start=True, stop=True), r=["CF", sp.key], w=[PF[bk].key])
            S.dve(lambda e: e.tensor_tensor(out=dd[:], in0=PF[bk][:, 0:4], in1=g[:, li0:li0 + 4], op=ALU.add), r=[PF[bk].key, g.key], w=[dd.key])
            S.act(lambda e: e.activation(out=nbt[:], in_=PF[bk][:, 0:4], func=AF.Copy), r=[PF[bk].key], w=[nbt.key])
            S.act(lambda e: e.activation(out=btot[:], in_=PF[bk][:, 4:8], func=AF.Copy), r=[PF[bk].key], w=[btot.key])
            S.dve(lambda e: e.tensor_tensor(out=rr[:], in0=IDB.raw(0, 128, 0, [[0, 4], [1, 128]]), in1=dd.raw(0, 128, 0, [[1, 4], [0, 128]]), op=ALU.mult),
                  r=["IDB", dd.key], w=[rr.key])
            bk2 = bank()
            S.pe(lambda e: e.matmul(PF[bk2][:, :], lhsT=ONESB[:], rhs=rr[:], start=True, stop=True), r=["ONESB", rr.key], w=[PF[bk2].key])
            S.dve(lambda e: e.tensor_reduce(out=mxb[:], in_=PF[bk2].raw(0, 128, 0, [[128, 4], [1, 128]]), axis=AX.X, op=ALU.max),
                  r=[PF[bk2].key], w=[mxb.key])
            S.dve(lambda e: e.tensor_tensor(out=wloc[:], in0=dd[:], in1=mxb[:], op=ALU.subtract), r=[dd.key, mxb.key], w=[wloc.key])
            S.act(lambda e: e.activation(out=wloc[:], in_=wloc[:], func=AF.Exp), r=[wloc.key], w=[wloc.key])

        def chain(i, d):
            j = i % NB
            mxb, btot, wloc, nbt = MXB[j], BTOT[j], WLOC[j], NBT[j]
            mm, dm, ae, ws, fl = MM[j], DM[j], AE[j], WS[j], FL[j]
            mst = MST[d]
            S.dve(lambda e: e.tensor_tensor(out=mm[:], in0=mst[:], in1=mxb[:], op=ALU.max), r=[mst.key, mxb.key], w=[mm.key])
            S.dve(lambda e: e.tensor_tensor(out=dm[:, 0, :], in0=mst[:], in1=mm[:], op=ALU.subtract), r=[mst.key, mm.key], w=[dm.key])
            S.dve(lambda e: e.tensor_tensor(out=dm[:, 1, :], in0=mxb[:], in1=mm[:], op=ALU.subtract), r=[mxb.key, mm.key], w=[dm.key])
            S.act(lambda e: e.activation(out=ae[:], in_=dm[:], func=AF.Exp), r=[dm.key], w=[ae.key])
            S.dve(lambda e: e.tensor_tensor(out=mst[:], in0=mm[:], in1=btot[:], op=ALU.subtract), r=[mm.key, btot.key], w=[mst.key])
            S.dve(lambda e: e.tensor_tensor(out=ws[:], in0=wloc[:], in1=ae[:, 1, :], op=ALU.mult), r=[wloc.key, ae.key], w=[ws.key])
            S.dve(lambda e: e.tensor_tensor(out=fl[:], in0=nbt[:], in1=mm[:], op=ALU.subtract), r=[nbt.key, mm.key], w=[fl.key])
            S.act(lambda e: e.activation(out=fl[:], in_=fl[:], func=AF.Exp), r=[fl.key], w=[fl.key])

        def state_local(i, bkk, bkv, kcol0=0):
            j = i % NB
            ksb, wv, wloc = KSB[j], WV[j], WLOC[j]
            S.act(lambda e: e.activation(out=ksb[:], in_=PF[bkk][:, kcol0:kcol0 + 256], func=AF.Copy, scale=0.125), r=[PF[bkk].key], w=[ksb.key])
            S.dve(lambda e: e.tensor_tensor(out=wv[:, :, 0:128], in0=PF[bkv].raw(0, 128, 0, [[128, 4], [1, 128]]),
                                            in1=wloc.raw(0, 128, 0, [[1, 4], [0, 128]]), op=ALU.mult),
                  r=[PF[bkv].key, wloc.key], w=[wv.key])
            S.dve(lambda e: e.tensor_copy(out=wv[:, :, 128:129], in_=wloc.raw(0, 128, 0, [[1, 4], [1, 1]])), r=[wloc.key], w=[wv.key])
            b0, b1 = bank(), bank()
            for h in range(4):
                bk = b0 if h < 2 else b1
                S.pe(lambda e, h=h, bk=bk: e.matmul(PF[bk][0:64, (h % 2) * 256:(h % 2) * 256 + 129], lhsT=ksb[:, h * 64:(h + 1) * 64],
                                                    rhs=wv[:, h, 0:129], start=True, stop=True),
                     r=[ksb.key, wv.key], w=[PF[bk].key])
            return b0, b1

        def state_update(i, d, b0, b1):
            j = i % NB
            ae, tm, le = AE[j], TMPS[j], LE[j]
            cst = CST[d]
            S.dve(lambda e: e.tensor_tensor(out=tm[:, :, 0:129], in0=cst[:, :, 0:129], in1=ae.raw(0, 64, 0, [[1, 4], [0, 129]]), op=ALU.mult),
                  r=[cst.key, ae.key], w=[tm.key])
            for hh, bk in enumerate((b0, b1)):
                S.dve(lambda e, hh=hh, bk=bk: e.tensor_tensor(out=le[:, 2 * hh:2 * hh + 2, 0:129], in0=PF[bk].raw(0, 64, 0, [[256, 2], [1, 129]]),
                                                              in1=ae.raw(0, 64, 4 + 2 * hh, [[1, 2], [0, 129]]), op=ALU.mult),
                      r=[PF[bk].key, ae.key], w=[le.key])
            S.pool(lambda e: e.tensor_tensor(out=cst[:, :, 0:129], in0=tm[:, :, 0:129], in1=le[:, :, 0:129], op=ALU.add),
                   r=[tm.key, le.key], w=[cst.key])

        steps = [("c", 0, 0), ("c", 1, 0), ("c", 1, 1), ("c", 0, 1)]
        steps += [("o", c, 1) for c in range(NCH - 1, -1, -1)]
        steps += [("l", c, 1) for c in range(NCH - 1, -1, -1)]
        if phases < 1:
            steps = []
        steps = steps[:int(os.environ.get("NSTEPS", 999))]
        for t_ in (TMPSB[0], TMPSB[1], TMPS[0], LE[0], WV[0], WV[1]):
            S.pool(lambda e, t_=t_: e.memset(t_[:], 0.0), w=[t_.key])

        def state_step(si, src, c, d):
            xt = XT[si % 2]
            srcap = {"c": cx, "o": xO, "l": xL}[src]
            S.dma("sp", lambda e, xt=xt, srcap=srcap, c=c: e.dma_start(out=xt[:], in_=srcap[c * 128:(c + 1) * 128, :]), "xt%d" % (si % 2), w=[xt.key])
            hT = norm_to_hxT(si, xt, A1c if src == "c" else A1x, S1c if src == "c" else S1x)
            bkk, bkv = bank(), bank()
            proj_tok(hT, bkk, CK, 272, gate_bias=True)
            proj_tok(hT, bkv, CV, 512)
            gate_pack(si, bkk, d)
            chain(si, d)
            b0, b1 = state_local(si, bkk, bkv)
            state_update(si, d, b0, b1)
            if src == "l":
                j = si % NB
                LSK = os.environ.get("LSKIP", "")
                if "a" not in LSK:
                    S.act(lambda e, j=j: e.activation(out=TMPSB[j][:, :, 0:129], in_=TMPS[j][:, :, 0:129], func=AF.Copy), r=[TMPS[j].key], w=[TMPSB[j].key])
                if "b" not in LSK:
                    S.dma("sp", lambda e, j=j, c=c: e.dma_start(out=tmpbs[c], in_=TMPSB[j][:]), "tmpb_st%d" % j, r=[TMPSB[j].key], w=["tmpbs%d" % c])
                if "c" not in LSK:
                    S.pool(lambda e, j=j, c=c: e.tensor_copy(out=WBs[:, c, :], in_=WS[j][:]), r=[WS[j].key], w=["WBs"])
                    S.pool(lambda e, j=j, c=c: e.tensor_copy(out=FLBs[:, c, :], in_=FL[j][:]), r=[FL[j].key], w=["FLBs"])
            if debug and si == 1:
                dbg("cstA", CST[0], [64, 520], lambda: CST[0][:])
                dbg("mstA", MST[0], [128, 4], lambda: MST[0][:])
            if debug and si == 3:
                dbg("cstB", CST[1], [64, 520], lambda: CST[1][:])
                dbg("mstB", MST[1], [128, 4], lambda: MST[1][:])

        SPB = [int(v) for v in os.environ.get("SPBAR", "").split(",") if v]
        for si, (src, c, d) in enumerate(steps):
            if si in SPB:
                S.barrier()
            state_step(si, src, c, d)
        if debug:
            dbg("cstBend", CST[1], [64, 520], lambda: CST[1][:])
            dbg("WBs", WBs, [128, NCH * 4], lambda: WBs[:])

        S.barrier()
        one = lambda shape, dt: wtiles(shape, dt, 1) * 2
        SIGO = one([128, 512], F32)
        G2 = SIGO
        VEXT = one([128, 4, 130], BF16)
        QT = one([64, 4, 128], BF16)
        KT = one([64, 4, 128], BF16)
        SPR = one([128, 2, 4, 128], BF16)
        TB = wtiles([64, 4, 130], BF16)
        TA = one([64, 4, 130], BF16)
        DEN = one([128, 8], F32)
        FLAB = one([128, 8], F32)
        HN = one([128, 8, 128], F32)
        HFT = HN
        HH = one([128, 512], F32)
        HSQ = one([128, 512], F32)
        SIGB = HSQ
        MO = one([128, 512], BF16)
        MT = one([128, 4, 128], BF16)
        UPAD = one([128, 4, 2, 94], BF16)
        YB = one([128, 4, 128], F32)
        YSQ = one([128, 4, 128], F32)
        MEAN = one([128, 128], F32)
        M2 = one([128, 128], F32)
        RSTC = one([128, 128], F32)
        CVT = one([128, 4, 128], BF16)
        HF = TMPF
        HFB = HXB
        HT2 = HXT
        SS = one([128, NE], F32)
        SBI = one([128, NE], F32)
        M8 = one([128, 8], F32)
        MSK = one([128, NE], F32)
        RS = one([128, 2], F32)
        for t_ in (TA[0], TB[0], TB[1]):
            S.pool(lambda e, t_=t_: e.memset(t_[:], 0.0), w=[t_.key])
        for j in range(1):
            S.pool(lambda e, j=j: e.memset(UPAD[j][:], 0.0), w=[UPAD[j].key])
            S.pool(lambda e, j=j: e.memset(VEXT[j][:], 1.0), w=[VEXT[j].key])
        print("arena top (mixer):", AR.top)

        nmain = int(os.environ.get('NMAIN', NCH)) if phases >= 2 else 0
        STG = int(os.environ.get('MAINSTAGE', 99))
        def main_chunk(c):
            j = c % NB
            xt = XT[c % 2]
            if os.environ.get("CHUNKBAR"):
                S.barrier()
            S.dma("sp", lambda e, xt=xt, c=c: e.dma_start(out=xt[:], in_=xL[c * 128:(c + 1) * 128, :]), "xt%d" % (c % 2), w=[xt.key])
            S.dma("sp", lambda e, j=j, c=c: e.dma_start(out=TB[j][:], in_=tmpbs[c]), "tb%d" % j, r=["tmpbs%d" % c], w=[TB[j].key])
            hT = norm_to_hxT(c, xt, A1x, S1x)
            bkk, bkv, bko = bank(), bank(), bank()
            proj_tok(hT, bkk, CK, 272, gate_bias=True)
            proj_tok(hT, bkv, CV, 512)
            proj_tok(hT, bko, CO, 512)
            bkq, bkt = bank(), bank()
            for h in range(4):
                proj_feat(hT, bkq, h * 128, CQ + h * 64, 64)
            for h in range(4):
                proj_feat(hT, bkt, h * 128, CK + h * 64, 64)
            if STG < 1:
                return
            S.act(lambda e, j=j: e.activation(out=QT[j][:], in_=PF[bkq][0:64, :], func=AF.Copy), r=[PF[bkq].key], w=[QT[j].key])
            S.act(lambda e, j=j: e.activation(out=KT[j][:], in_=PF[bkt][0:64, :], func=AF.Copy, scale=0.125), r=[PF[bkt].key], w=[KT[j].key])
            S.act(lambda e, j=j: e.activation(out=SIGO[j][:], in_=PF[bko][:, :], func=AF.Sigmoid), r=[PF[bko].key], w=[SIGO[j].key])
            S.dve(lambda e, j=j: e.tensor_copy(out=VEXT[j][:, :, 0:128], in_=PF[bkv].raw(0, 128, 0, [[128, 4], [1, 128]])), r=[PF[bkv].key], w=[VEXT[j].key])
            if STG < 2:
                return
            gate_pack(c, bkk, 0)
            chain(c, 0)
            b0, b1 = state_local(c, bkk, bkv)
            state_update(c, 0, b0, b1)
            S.act(lambda e, j=j: e.activation(out=TA[j][:, :, 0:129], in_=TMPS[j][:, :, 0:129], func=AF.Copy), r=[TMPS[j].key], w=[TA[j].key])
            if STG < 3:
                return
            bks = bank()
            for h in range(4):
                S.pe(lambda e, h=h, j=j: e.matmul(PF[bks][:, h * 128:(h + 1) * 128], lhsT=KT[j][:, h, :], rhs=QT[j][:, h, :], start=True, stop=True),
                     r=[KT[j].key, QT[j].key], w=[PF[bks].key])
            for h in range(4):
                S.dve(lambda e, h=h, j=j: e.scalar_tensor_tensor(out=SPR[j][:, 0, h, :], in0=PF[bks][:, h * 128:(h + 1) * 128], scalar=WS[j][:, h:h + 1],
                                                                 in1=MA(), op0=ALU.mult, op1=ALU.mult),
                      r=[PF[bks].key, WS[j].key, "CF"], w=[SPR[j].key])
                S.dve(lambda e, h=h, j=j, c=c: e.scalar_tensor_tensor(out=SPR[j][:, 1, h, :], in0=PF[bks][:, h * 128:(h + 1) * 128], scalar=WBs[:, c, h:h + 1],
                                                                      in1=MB(), op0=ALU.mult, op1=ALU.mult),
                      r=[PF[bks].key, "WBs", "CF"], w=[SPR[j].key])
            if STG < 4:
                return
            nb_ = [bank() for _ in range(4)]
            for d in range(2):
                for h in range(4):
                    bk = nb_[d * 2 + h // 2]
                    o0 = (h % 2) * 256
                    tt = TA[j] if d == 0 else TB[j]
                    S.pe(lambda e, d=d, h=h, bk=bk, o0=o0, j=j: e.matmul(PF[bk][:, o0:o0 + 129], lhsT=SPR[j][:, d, h, :], rhs=VEXT[j][:, h, 0:129], start=True, stop=False),
                         r=[SPR[j].key, VEXT[j].key], w=[PF[bk].key])
                    S.pe(lambda e, h=h, bk=bk, o0=o0, j=j, tt=tt: e.matmul(PF[bk][:, o0:o0 + 129], lhsT=QT[j][:, h, :], rhs=tt[:, h, 0:129], start=False, stop=True),
                         r=[QT[j].key, tt.key], w=[PF[bk].key])
            if STG < 5:
                return
            S.pool(lambda e, j=j: e.tensor_copy(out=FLAB[j][:, 0:4], in_=FL[j][:]), r=[FL[j].key], w=[FLAB[j].key])
            S.pool(lambda e, j=j, c=c: e.tensor_copy(out=FLAB[j][:, 4:8], in_=FLBs[:, c, :]), r=["FLBs"], w=[FLAB[j].key])
            for q4 in range(4):
                bk = nb_[q4]
                S.dve(lambda e, q4=q4, bk=bk, j=j: e.scalar_tensor_tensor(out=DEN[j][:, 2 * q4:2 * q4 + 2], in0=PF[bk].raw(0, 128, 128, [[256, 2]]), scalar=-1.0,
                                                                         in1=FLAB[j][:, 2 * q4:2 * q4 + 2], op0=ALU.mult, op1=ALU.max),
                      r=[PF[bk].key, FLAB[j].key], w=[DEN[j].key])
                S.dve(lambda e, q4=q4, bk=bk, j=j: e.tensor_tensor(out=DEN[j][:, 2 * q4:2 * q4 + 2], in0=PF[bk].raw(0, 128, 128, [[256, 2]]),
                                                                  in1=DEN[j][:, 2 * q4:2 * q4 + 2], op=ALU.max),
                      r=[PF[bk].key, DEN[j].key], w=[DEN[j].key])
            S.dve(lambda e, j=j: e.reciprocal(out=DEN[j][:], in_=DEN[j][:]), r=[DEN[j].key], w=[DEN[j].key])
            for q4 in range(4):
                bk = nb_[q4]
                S.dve(lambda e, q4=q4, bk=bk, j=j: e.tensor_tensor(out=HN[j][:, 2 * q4:2 * q4 + 2, :], in0=PF[bk].raw(0, 128, 0, [[256, 2], [1, 128]]),
                                                                  in1=DEN[j].raw(0, 128, 2 * q4, [[1, 2], [0, 128]]), op=ALU.mult),
                      r=[PF[bk].key, DEN[j].key], w=[HN[j].key])
            S.pool(lambda e, j=j: e.tensor_tensor(out=HH[j][:], in0=HN[j][:, 0:4, :], in1=HN[j][:, 4:8, :], op=ALU.add), r=[HN[j].key], w=[HH[j].key])
            S.pool(lambda e, j=j: e.tensor_tensor(out=HSQ[j][:], in0=HH[j][:], in1=HH[j][:], op=ALU.mult), r=[HH[j].key], w=[HSQ[j].key])
            S.dve(lambda e, j=j: e.tensor_reduce(out=SSQ[j][:, 0:4], in_=HSQ[j].raw(0, 128, 0, [[128, 4], [1, 128]]), axis=AX.X, op=ALU.add),
                  r=[HSQ[j].key], w=[SSQ[j].key])
            finish_rstd(c, 0, 4, 1.0 / 128)
            S.pool(lambda e, j=j: e.tensor_tensor(out=G2[j][:], in0=SIGO[j][:], in1=NGB[:], op=ALU.mult), r=[SIGO[j].key, "NGB"], w=[G2[j].key])
            S.dve(lambda e, j=j: e.tensor_tensor(out=HH[j][:], in0=HH[j].raw(0, 128, 0, [[128, 4], [1, 128]]), in1=SSQ[j].raw(0, 128, 0, [[1, 4], [0, 128]]), op=ALU.mult),
                  r=[HH[j].key, SSQ[j].key], w=[HH[j].key])
            S.pool(lambda e, j=j: e.tensor_tensor(out=MO[j][:], in0=HH[j][:], in1=G2[j][:], op=ALU.mult), r=[HH[j].key, G2[j].key], w=[MO[j].key])
            bk = bank()
            for k in range(4):
                S.pe(lambda e, k=k, bk=bk, j=j: e.transpose(out=PB[bk][:, k * 128:(k + 1) * 128], in_=MO[j][:, k * 128:(k + 1) * 128], identity=IDB[:]),
                     r=[MO[j].key, "IDB"], w=[PB[bk].key])
            S.act(lambda e, bk=bk, j=j: e.activation(out=MT[j][:], in_=PB[bk][:, 0:512], func=AF.Copy), r=[PB[bk].key], w=[MT[j].key])
            if STG < 6:
                return
            bka, bkb = bank(), bank()
            for cc in range(4):
                proj_feat(hT, bka, cc * 128, CGA + cc * 128, 128)
            for cc in range(4):
                proj_feat(hT, bkb, cc * 128, CGB + cc * 128, 128)
            S.act(lambda e, j=j: e.activation(out=SIGB[j][:], in_=PF[bkb][:, :], func=AF.Sigmoid), r=[PF[bkb].key], w=[SIGB[j].key])
            for cc in range(4):
                S.dve(lambda e, cc=cc, j=j: e.tensor_tensor(out=UPAD[j].raw(0, 128, cc * 188 + 15, [[94, 2], [1, 64]]), in0=PF[bka].raw(0, 128, cc * 128, [[64, 2], [1, 64]]),
                                                           in1=SIGB[j].raw(0, 128, cc * 128, [[64, 2], [1, 64]]), op=ALU.mult),
                      r=[PF[bka].key, SIGB[j].key], w=[UPAD[j].key])
            bky = bank()
            for cc in range(4):
                for t in range(31):
                    S.pe(lambda e, cc=cc, t=t, j=j: e.matmul(PF[bky][:, cc * 128:(cc + 1) * 128], lhsT=DIAG[:, cc * 31 + t, :],
                                                             rhs=UPAD[j].raw(0, 128, cc * 188 + t, [[94, 2], [1, 64]]), start=(t == 0), stop=(t == 30)),
                         r=["DIAG", UPAD[j].key], w=[PF[bky].key])
            for cc in range(4):
                S.act(lambda e, cc=cc, j=j: e.activation(out=YB[j][:, cc, :], in_=PF[bky][:, cc * 128:(cc + 1) * 128], func=AF.Identity, bias=CONVP[:, cc, 31:32]),
                      r=[PF[bky].key, "CONVP"], w=[YB[j].key])
            S.pool(lambda e, j=j: e.tensor_tensor(out=YSQ[j][:], in0=YB[j][:], in1=YB[j][:], op=ALU.mult), r=[YB[j].key], w=[YSQ[j].key])
            bkm = bank()
            for cc in range(4):
                S.pe(lambda e, cc=cc, j=j: e.matmul(PF[bkm][:, 0:128], lhsT=ONES512(), rhs=YB[j][:, cc, :], start=(cc == 0), stop=(cc == 3)),
                     r=["CF", YB[j].key], w=[PF[bkm].key])
            for cc in range(4):
                S.pe(lambda e, cc=cc, j=j: e.matmul(PF[bkm][:, 128:256], lhsT=ONES512(), rhs=YSQ[j][:, cc, :], start=(cc == 0), stop=(cc == 3)),
                     r=["CF", YSQ[j].key], w=[PF[bkm].key])
            S.act(lambda e, j=j: e.activation(out=MEAN[j][:], in_=PF[bkm][:, 0:128], func=AF.Copy), r=[PF[bkm].key], w=[MEAN[j].key])
            S.act(lambda e, j=j: e.activation(out=M2[j][:], in_=PF[bkm][:, 0:128], func=AF.Square), r=[PF[bkm].key], w=[M2[j].key])
            S.dve(lambda e, j=j: e.tensor_tensor(out=RSTC[j][:], in0=PF[bkm][:, 128:256], in1=M2[j][:], op=ALU.subtract), r=[PF[bkm].key, M2[j].key], w=[RSTC[j].key])
            S.act(lambda e, j=j: e.activation(out=RSTC[j][:], in_=RSTC[j][:], func=AF.Sqrt, bias=EPS), r=[RSTC[j].key], w=[RSTC[j].key])
            S.dve(lambda e, j=j: e.reciprocal(out=RSTC[j][:], in_=RSTC[j][:]), r=[RSTC[j].key], w=[RSTC[j].key])
            S.pool(lambda e, j=j: e.tensor_tensor(out=YB[j][:], in0=YB[j][:], in1=MEAN[j].raw(0, 128, 0, [[0, 4], [1, 128]]), op=ALU.subtract),
                   r=[YB[j].key, MEAN[j].key], w=[YB[j].key])
            S.pool(lambda e, j=j: e.tensor_tensor(out=YB[j][:], in0=YB[j][:], in1=RSTC[j].raw(0, 128, 0, [[0, 4], [1, 128]]), op=ALU.mult),
                   r=[YB[j].key, RSTC[j].key], w=[YB[j].key])
            for cc in range(4):
                S.act(lambda e, cc=cc, j=j: e.activation(out=CVT[j][:, cc, :], in_=YB[j][:, cc, :], func=AF.Silu, scale=CONVP[:, cc, 32:33], bias=CONVP[:, cc, 33:34]),
                      r=[YB[j].key, "CONVP"], w=[CVT[j].key])
            if STG < 7:
                return
            by = [bank(), bank()]
            for hh in range(2):
                for k in range(8):
                    src = MT[j] if k < 4 else CVT[j]
                    S.pe(lambda e, hh=hh, k=k, src=src: e.matmul(PF[by[hh]][:, :], lhsT=src[:, k % 4, :], rhs=WOUT[:, k, hh * 512:(hh + 1) * 512], start=(k == 0), stop=False),
                         r=[src.key, "WOUT"], w=[PF[by[hh]].key])
                S.pe(lambda e, hh=hh: e.matmul(PF[by[hh]][:, :], lhsT=ONER[0:1, 0:128], rhs=BOUTR[0:1, hh * 512:(hh + 1) * 512], start=False, stop=True),
                     r=["ONER", "BOUTR"], w=[PF[by[hh]].key])
            if STG < 8:
                return
            rms_rstd(c, lambda: PF[by[0]][:, :], [PF[by[0]].key], 512, 2)
            rms_rstd(c, lambda: PF[by[1]][:, :], [PF[by[1]].key], 512, 3)
            S.dve(lambda e, j=j: e.tensor_tensor(out=SSQ[j][:, 2:3], in0=SSQ[j][:, 2:3], in1=SSQ[j][:, 3:4], op=ALU.add), r=[SSQ[j].key], w=[SSQ[j].key])
            finish_rstd(c, 2, 1, 1.0 / D)
            for hh in range(2):
                S.dve(lambda e, hh=hh, j=j: e.scalar_tensor_tensor(out=TMPF[j][:, hh * 512:(hh + 1) * 512], in0=PF[by[hh]][:, :], scalar=SSQ[j][:, 2:3],
                                                                   in1=A2x[:, hh * 512:(hh + 1) * 512], op0=ALU.mult, op1=ALU.mult),
                      r=[PF[by[hh]].key, SSQ[j].key, "A2x"], w=[TMPF[j].key])
            S.pool(lambda e, j=j, xt=xt: e.tensor_tensor(out=xt[:], in0=xt[:], in1=TMPF[j][:], op=ALU.add), r=[xt.key, TMPF[j].key], w=[xt.key])
            S.dma("sp", lambda e, xt=xt, c=c: e.dma_start(out=x1s[c * 128:(c + 1) * 128, :], in_=xt[:]), "x1st%d" % (c % 2), r=[xt.key], w=["x1s%d" % c])
            if debug and c == 0:
                dbg("hh0", HH[j], [128, 512], lambda: HH[0][:])
                dbg("x1_0", xt, [128, D], lambda: XT[0][:])
                dbg("cvt0", CVT[j], [128, 512], lambda: CVT[0][:])
                dbg("mo0", MO[j], [128, 512], lambda: MO[0][:])
            if STG < 9:
                return
            rms_rstd(c, lambda xt=xt: xt[:], [xt.key], D, 1)
            finish_rstd(c, 1, 1, 1.0 / D)
            S.dve(lambda e, j=j, xt=xt: e.scalar_tensor_tensor(out=TMPF[j][:], in0=xt[:], scalar=SSQ[j][:, 1:2], in1=A3x[:], op0=ALU.mult, op1=ALU.mult),
                  r=[xt.key, SSQ[j].key, "A3x"], w=[TMPF[j].key])
            S.pool(lambda e, j=j: e.tensor_tensor(out=HF[j][:], in0=TMPF[j][:], in1=S3x[:], op=ALU.add), r=[TMPF[j].key, "S3x"], w=[HF[j].key])
            S.pool(lambda e, j=j: e.tensor_copy(out=HFB[j][:], in_=HF[j][:]), r=[HF[j].key], w=[HFB[j].key])
            bk = bank()
            for k in range(8):
                S.pe(lambda e, k=k, bk=bk, j=j: e.transpose(out=PB[bk][:, k * 128:(k + 1) * 128], in_=HFB[j][:, k * 128:(k + 1) * 128], identity=IDB[:]),
                     r=[HFB[j].key, "IDB"], w=[PB[bk].key])
            S.act(lambda e, bk=bk, j=j: e.activation(out=HT2[j][:], in_=PB[bk][:, :], func=AF.Copy), r=[PB[bk].key], w=[HT2[j].key])
            S.dma("sp", lambda e, j=j, c=c: e.dma_start(out=hts.rearrange("p (k t) -> p k t", k=8)[:, :, c * 128:(c + 1) * 128], in_=HT2[j][:]),
                  "htst%d" % j, r=[HT2[j].key], w=["hts%d" % (c // 16)])
            bf = [bank(), bank()]
            for k in range(8):
                S.pe(lambda e, k=k, j=j: e.transpose(out=PF[bf[k // 4]][:, (k % 4) * 128:(k % 4 + 1) * 128], in_=HF[j][:, k * 128:(k + 1) * 128], identity=IDF()),
                     r=[HF[j].key, "CF"], w=[PF[bf[k // 4]].key])
            S.act(lambda e, j=j: e.activation(out=HFT[j][:, 0:4, :], in_=PF[bf[0]][:, :], func=AF.Copy), r=[PF[bf[0]].key], w=[HFT[j].key])
            S.dve(lambda e, j=j: e.tensor_copy(out=HFT[j][:, 4:8, :], in_=PF[bf[1]][:, :]), r=[PF[bf[1]].key], w=[HFT[j].key])
            bkr = bank()
            for k in range(8):
                S.pe(lambda e, k=k, j=j: e.matmul(PF[bkr][:, 0:NE], lhsT=HFT[j][:, k, :], rhs=WRF[:, k, :], start=(k == 0), stop=(k == 7)),
                     r=[HFT[j].key, "WRF"], w=[PF[bkr].key])
            S.act(lambda e, j=j: e.activation(out=SS[j][:], in_=PF[bkr][:, 0:NE], func=AF.Sigmoid), r=[PF[bkr].key], w=[SS[j].key])
            S.dve(lambda e, j=j: e.tensor_tensor(out=SBI[j][:], in0=SS[j][:], in1=BRB[:], op=ALU.add), r=[SS[j].key, "BRB"], w=[SBI[j].key])
            S.dve(lambda e, j=j: e.max(out=M8[j][:], in_=SBI[j][:]), r=[SBI[j].key], w=[M8[j].key])
            S.dve(lambda e, j=j: e.tensor_scalar(out=MSK[j][:], in0=SBI[j][:], scalar1=M8[j][:, 7:8], scalar2=None, op0=ALU.is_ge), r=[SBI[j].key, M8[j].key], w=[MSK[j].key])
            S.dve(lambda e, j=j: e.tensor_tensor(out=MSK[j][:], in0=MSK[j][:], in1=SS[j][:], op=ALU.mult), r=[MSK[j].key, SS[j].key], w=[MSK[j].key])
            S.dve(lambda e, j=j: e.tensor_reduce(out=RS[j][:, 0:1], in_=MSK[j][:], axis=AX.X, op=ALU.add), r=[MSK[j].key], w=[RS[j].key])
            S.dve(lambda e, j=j: e.reciprocal(out=RS[j][:, 1:2], in_=RS[j][:, 0:1]), r=[RS[j].key], w=[RS[j].key])
            S.dve(lambda e, j=j, c=c: e.tensor_scalar(out=WTS[:, c, 0:NE], in0=MSK[j][:], scalar1=RS[j][:, 1:2], scalar2=2.5, op0=ALU.mult, op1=ALU.mult),
                  r=[MSK[j].key, RS[j].key], w=["WTS"])

        for c in range(nmain):
            main_chunk(c)
        if debug:
            dbg("WTS", WTS, [128, NCH * (NE + 1)], lambda: WTS[:])
            t_ = nc.dram_tensor("dbg_x1all", [NL, D], F32, kind="ExternalOutput").ap()
            dbg_out["x1all"] = [NL, D]
            S.dma("sp", lambda e: e.dma_start(out=t_, in_=x1s), "dbg_x1all", r=["x1s%d" % c for c in range(nmain)])

        S.barrier()
        AR.release(persist_mark)
        ACC = AR.alloc([128, 16, D], F32, "ACC")
        HTM = AR.alloc([128, 8, 2048], BF16, "HTM")
        WGU = [AR.alloc([128, 8, 512], BF16) for _ in range(2)]
        WDN = [AR.alloc([128, 2, D], BF16) for _ in range(2)]
        SG = [AR.alloc([128, 256], F32) for _ in range(2)]
        ACT_ = [AR.alloc([128, 256], BF16) for _ in range(2)]
        ACTT = [AR.alloc([128, 2, 128], BF16) for _ in range(2)]
        X1T = [AR.alloc([128, D], F32) for _ in range(2)]
        TF2 = [AR.alloc([128, D], F32) for _ in range(2)]
        JK2 = AR.alloc([128, D], BF16)
        SQ2 = [AR.alloc([128, 2], F32) for _ in range(2)]
        print("arena top (moe):", AR.top)
        nexp = NE + 1 if phases >= 3 else 0
        u = 0
        for half in range(2 if phases >= 3 else 0):
            S.dma("sp", lambda e, half=half: e.dma_start(out=HTM[:], in_=hts.rearrange("p (k t) -> p k t", k=8)[:, :, half * 2048:(half + 1) * 2048]),
                  "htm", r=["hts%d" % half], w=["HTM"])
            for ex in range(nexp):
                wb = ex % 2
                S.dma("pool", lambda e, ex=ex, wb=wb: e.dma_start(out=WGU[wb][:], in_=we_gu[ex].rearrange("(k p) n -> p k n", p=128)), "wgu%d" % wb, w=[WGU[wb].key])
                S.dma("pool", lambda e, ex=ex, wb=wb: e.dma_start(out=WDN[wb][:], in_=we_d[ex].rearrange("(k p) n -> p k n", p=128)), "wdn%d" % wb, w=[WDN[wb].key])
                for i in range(16):
                    ti = half * 16 + i
                    jj = u % 2
                    u += 1
                    bg = bank()
                    for k in range(8):
                        S.pe(lambda e, k=k, i=i, wb=wb, bg=bg: e.matmul(PF[bg][:, :], lhsT=HTM[:, k, i * 128:(i + 1) * 128], rhs=WGU[wb][:, k, :], start=(k == 0), stop=(k == 7)),
                             r=["HTM", WGU[wb].key], w=[PF[bg].key])
                    S.act(lambda e, jj=jj, bg=bg: e.activation(out=SG[jj][:], in_=PF[bg][:, 0:256], func=AF.Silu), r=[PF[bg].key], w=[SG[jj].key])
                    S.dve(lambda e, jj=jj, bg=bg, ti=ti, ex=ex: e.scalar_tensor_tensor(out=ACT_[jj][:], in0=PF[bg][:, 256:512], scalar=WTS[:, ti, ex:ex + 1], in1=SG[jj][:],
                                                                                 op0=ALU.mult, op1=ALU.mult),
                          r=[PF[bg].key, "WTS", SG[jj].key], w=[ACT_[jj].key])
                    bt = bank()
                    for cc in range(2):
                        S.pe(lambda e, cc=cc, jj=jj, bt=bt: e.transpose(out=PB[bt][:, cc * 128:(cc + 1) * 128], in_=ACT_[jj][:, cc * 128:(cc + 1) * 128], identity=IDB[:]),
                             r=[ACT_[jj].key, "IDB"], w=[PB[bt].key])
                    S.act(lambda e, jj=jj, bt=bt: e.activation(out=ACTT[jj][:], in_=PB[bt][:, 0:256], func=AF.Copy), r=[PB[bt].key], w=[ACTT[jj].key])
                    byy = [bank(), bank()]
                    for hh in range(2):
                        for cc in range(2):
                            S.pe(lambda e, hh=hh, cc=cc, jj=jj, wb=wb, byy=byy: e.matmul(PF[byy[hh]][:, :], lhsT=ACTT[jj][:, cc, :], rhs=WDN[wb][:, cc, hh * 512:(hh + 1) * 512],
                                                                                start=(cc == 0), stop=(cc == 1)),
                                 r=[ACTT[jj].key, WDN[wb].key], w=[PF[byy[hh]].key])
                    for hh in range(2):
                        if ex == 0:
                            S.act(lambda e, hh=hh, i=i, byy=byy: e.activation(out=ACC[:, i, hh * 512:(hh + 1) * 512], in_=PF[byy[hh]][:, :], func=AF.Copy),
                                  r=[PF[byy[hh]].key], w=["ACC%d" % i])
                        else:
                            S.dve(lambda e, hh=hh, i=i, byy=byy: e.tensor_tensor(out=ACC[:, i, hh * 512:(hh + 1) * 512], in0=ACC[:, i, hh * 512:(hh + 1) * 512], in1=PF[byy[hh]][:, :], op=ALU.add),
                                  r=[PF[byy[hh]].key, "ACC%d" % i], w=["ACC%d" % i])
            for i in range(16):
                ti = half * 16 + i
                jj = i % 2
                S.dma("sp", lambda e, jj=jj, ti=ti: e.dma_start(out=X1T[jj][:], in_=x1s[ti * 128:(ti + 1) * 128, :]), "x1l%d" % jj, r=["x1s%d" % ti], w=[X1T[jj].key])
                S.act(lambda e, jj=jj, i=i: e.activation(out=JK2[:], in_=ACC[:, i, :], func=AF.Square, accum_out=SQ2[jj][:, 0:1]), r=["ACC%d" % i], w=[JK2.key, SQ2[jj].key])
                S.act(lambda e, jj=jj: e.activation(out=SQ2[jj][:, 0:1], in_=SQ2[jj][:, 0:1], func=AF.Sqrt, scale=1.0 / D, bias=EPS), r=[SQ2[jj].key], w=[SQ2[jj].key])
                S.dve(lambda e, jj=jj: e.reciprocal(out=SQ2[jj][:, 1:2], in_=SQ2[jj][:, 0:1]), r=[SQ2[jj].key], w=[SQ2[jj].key])
                S.dve(lambda e, jj=jj, i=i: e.scalar_tensor_tensor(out=TF2[jj][:], in0=ACC[:, i, :], scalar=SQ2[jj][:, 1:2], in1=A4x[:], op0=ALU.mult, op1=ALU.mult),
                      r=["ACC%d" % i, SQ2[jj].key, "A4x"], w=[TF2[jj].key])
                S.pool(lambda e, jj=jj: e.tensor_tensor(out=TF2[jj][:], in0=TF2[jj][:], in1=X1T[jj][:], op=ALU.add), r=[TF2[jj].key, X1T[jj].key], w=[TF2[jj].key])
                S.dma("sp", lambda e, jj=jj, ti=ti: e.dma_start(out=out[ti * 128:(ti + 1) * 128, :], in_=TF2[jj][:]), "ost%d" % jj, r=[TF2[jj].key])
        S.emit(st)
    print("ops per engine:", S.stats, "sems:", S.n_sems, "est_ms:", getattr(S, "est_ns", 0) / 1e6, "barriers_ms:", [round(x / 1e6, 3) for x in getattr(S, "barrier_ns", [])], "busy_ms:", {k: round(v / 1e6, 2) for k, v in getattr(S, "busy_ns", {}).items()})
    return nc, dbg_out


def _core_inputs(b, s, x, c, ctx, c_ctx, w_ada, b_ada, g_pre_mix, g_post_mix, g_pre_ffn, g_post_ffn, w_in, b_in, b_gate,
                 mlstm_norm_g, conv_w, conv_b, conv_ln_g, conv_ln_b, w_out, b_out, w_router, b_router, shared):
    T = x.shape[1]
    half = T // 2
    if s == 0:
        xl = x[b, :half]
        xo = x[b, half:]
        cxx = ctx[b]
        gperm = list(range(16))
        cw = conv_w[0]
    else:
        xl = x[b, half:][::-1]
        xo = x[b, :half][::-1]
        cxx = ctx[b][::-1]
        gperm = list(range(8, 16)) + list(range(0, 8))
        cw = conv_w[0][::-1]
    cols = np.concatenate([np.arange(0, 256), np.arange(256, 512), 1536 + np.array(gperm), np.arange(512, 1024), np.arange(1024, 1536),
                           np.arange(1552, 2064), np.arange(2064, 2576)])
    c_t = np.concatenate([c[b].reshape(8, 128).T, c_ctx.reshape(8, 128).T], axis=1)
    convp = np.zeros((128, 4, 34), np.float32)
    convp[:, :, 0:31] = cw.reshape(31, 4, 128).transpose(2, 1, 0)
    convp[:, :, 31] = conv_b[0].reshape(4, 128).T
    convp[:, :, 32] = conv_ln_g[0].reshape(4, 128).T
    convp[:, :, 33] = conv_ln_b[0].reshape(4, 128).T
    d = dict(shared)
    d.update(
        xL=np.ascontiguousarray(xl), xO=np.ascontiguousarray(xo), cx=np.ascontiguousarray(cxx),
        c_t=np.ascontiguousarray(c_t, dtype=np.float32),
        w_in=np.ascontiguousarray(w_in[0][:, cols]), b_in=np.ascontiguousarray(b_in[0][cols][None, :]),
        b_gate=np.ascontiguousarray(b_gate[0].reshape(16)[gperm][None, :]),
        convp=np.ascontiguousarray(convp.reshape(128, 136)),
    )
    return d


def _shared_inputs(w_ada, b_ada, g_pre_mix, g_post_mix, g_pre_ffn, g_post_ffn, mlstm_norm_g, w_out, b_out, w_router, b_router,
                   we_gate, we_up, we_down, ws_gate, ws_up, ws_down):
    we_gu = np.empty((NE + 1, D, 512), np.float32)
    we_gu[:NE, :, :256] = we_gate[0]
    we_gu[:NE, :, 256:] = we_up[0]
    we_gu[NE, :, :256] = ws_gate[0]
    we_gu[NE, :, 256:] = ws_up[0]
    we_d = np.empty((NE + 1, 256, D), np.float32)
    we_d[:NE] = we_down[0]
    we_d[NE] = ws_down[0]
    idx = np.arange(128)
    consts = np.zeros((128, 5, 128), np.float32)
    consts[:, 0] = np.eye(128)
    consts[:, 1] = (idx[:, None] <= idx[None, :])
    consts[:, 2] = (idx[:, None] >= idx[None, :])
    consts[:, 3] = 1.0
    consts[:, 4] = 1.0 / 512
    sel = np.zeros((2, 256), np.float32)
    sel[0, :128] = 1.0
    sel[1, 128:] = 1.0
    return dict(
        w_ada=np.ascontiguousarray(w_ada[0]), b_ada=np.ascontiguousarray(b_ada),
        gvec=np.ascontiguousarray(np.stack([g_pre_mix[0], g_post_mix[0], g_pre_ffn[0], g_post_ffn[0]])),
        ng=np.ascontiguousarray(mlstm_norm_g[0].reshape(1, 512)),
        w_out=np.ascontiguousarray(w_out[0]), b_out=np.ascontiguousarray(b_out),
        w_r=np.ascontiguousarray(w_router[0]), b_r=np.ascontiguousarray(b_router),
        we_gu=we_gu, we_d=we_d, consts=consts.reshape(128, 640), sel=sel,
    )


_NC_CACHE = {}


def kernel(x, c, ctx, c_ctx, w_ada, b_ada, g_pre_mix, g_post_mix, g_pre_ffn, g_post_ffn,
           w_in, b_in, b_gate, mlstm_norm_g, conv_w, conv_b, conv_ln_g, conv_ln_b, w_out, b_out,
           w_router, b_router, we_gate, we_up, we_down, ws_gate, ws_up, ws_down):
    args = [np.asarray(a, dtype=np.float32) for a in (x, c, ctx, c_ctx, w_ada, b_ada, g_pre_mix, g_post_mix, g_pre_ffn, g_post_ffn,
                                                     w_in, b_in, b_gate, mlstm_norm_g, conv_w, conv_b, conv_ln_g, conv_ln_b, w_out, b_out,
                                                     w_router, b_router, we_gate, we_up, we_down, ws_gate, ws_up, ws_down)]
    (x, c, ctx, c_ctx, w_ada, b_ada, g_pre_mix, g_post_mix, g_pre_ffn, g_post_ffn, w_in, b_in, b_gate, mlstm_norm_g, conv_w, conv_b,
     conv_ln_g, conv_ln_b, w_out, b_out, w_router, b_router, we_gate, we_up, we_down, ws_gate, ws_up, ws_down) = args
    shared = _shared_inputs(w_ada, b_ada, g_pre_mix, g_post_mix, g_pre_ffn, g_post_ffn, mlstm_norm_g, w_out, b_out, w_router, b_router,
                            we_gate, we_up, we_down, ws_gate, ws_up, ws_down)
    in_maps = []
    for core in range(8):
        b, s = core // 2, core % 2
        in_maps.append(_core_inputs(b, s, x, c, ctx, c_ctx, w_ada, b_ada, g_pre_mix, g_post_mix, g_pre_ffn, g_post_ffn, w_in, b_in, b_gate,
                                    mlstm_norm_g, conv_w, conv_b, conv_ln_g, conv_ln_b, w_out, b_out, w_router, b_router, shared))
    if "nc" not in _NC_CACHE:
        _NC_CACHE["nc"] = build()[0]
    res = run_bass_kernel_spmd(_NC_CACHE["nc"], in_maps, core_ids=list(range(8)))
    B, T = x.shape[0], x.shape[1]
    half = T // 2
    outp = np.empty((B, T, D), np.float32)
    for core in range(8):
        b, s = core // 2, core % 2
        o = np.asarray(res.results[core]["out"])
        if s == 0:
            outp[b, :half] = o
        else:
            outp[b, half:] = o[::-1]
    return outp
```

```python
import os
import numpy as np
from contextlib import ExitStack
import concourse.bass as bass
import concourse.mybir as mybir
from concourse.bass_utils import run_bass_kernel_spmd

F32 = mybir.dt.float32
BF16 = mybir.dt.bfloat16
AF = mybir.ActivationFunctionType
ALU = mybir.AluOpType
AX = mybir.AxisListType

D = 1024
NL = 4096
NCH = NL // 128
CTX = 256
NE = 64
EPS = 1e-6
DIN = 2576
ARENA_WORDS = 53000


class Op:
    __slots__ = ("eng", "fn", "reads", "writes", "dma", "deps", "milestone", "mval", "msem", "pos", "dval", "waits")

    def __init__(self, eng, fn, reads, writes, dma):
        self.eng = eng
        self.fn = fn
        self.reads = tuple(reads)
        self.writes = tuple(writes)
        self.dma = dma
        self.milestone = False
        self.mval = 0
        self.msem = 0
        self.dval = 0
        self.waits = []


class _Recorder:
    def __getattr__(self, name):
        def f(*args, **kw):
            self.last = (name, args, kw)
            return self
        return f


class Sched:
    ENGS = ("pe", "act", "dve", "pool", "sp")
    CAP = 16000

    def __init__(self, nc):
        self.nc = nc
        self.ops = []
        self._barrier = False
        self.ps_touch = {}

    def add(self, eng, fn, r=(), w=(), dma=None):
        if self._barrier and fn is not None:
            r = tuple(r) + ("__barrier__",)
        op = Op(eng, fn, r, w, dma)
        self.ops.append(op)
        for k in op.reads + op.writes:
            if isinstance(k, str) and k.startswith("ps") and len(k) == 3:
                self.ps_touch[k] = len(self.ops)
        return op

    def pe(self, fn, r=(), w=()):
        return self.add("pe", fn, r, w)

    def act(self, fn, r=(), w=()):
        return self.add("act", fn, r, w)

    def dve(self, fn, r=(), w=()):
        return self.add("dve", fn, r, w)

    def pool(self, fn, r=(), w=()):
        return self.add("pool", fn, r, w)

    def dma(self, q, fn, key, r=(), w=()):
        return self.add(q, fn, r, w, dma=key)

    def barrier(self):
        self.add("sp", None, r=(), w=("__barrier__",), dma=None)
        self._barrier = True

    reorder = os.environ.get("NOREORDER") is None

    @staticmethod
    def _cost(op):
        if op.fn is None:
            return 50.0, 50.0
        rec = _Recorder()
        op.fn(rec)
        name, args, kw = rec.last

        def fsz(ap):
            n = 1
            for d in ap.shape[1:]:
                n *= d
            return n
        if name == "dma_start":
            o = kw.get("out")
            nbytes = fsz(o) * o.shape[0] * 4
            issue = 1200.0 if op.eng == "pool" else 120.0
            return issue, issue + 2200.0 + nbytes / 150.0
        if name == "matmul":
            n = fsz(kw["rhs"])
            c = max(n, 48) * 0.55 + 25
            if kw["rhs"].dtype == F32:
                c *= 4
            return c, c + 120
        if name == "transpose":
            c = 100.0 if kw["in_"].dtype != F32 else 160.0
            return c, c + 120
        if name in ("tensor_reduce", "max"):
            n = fsz(kw["in_"])
        elif name == "memset":
            n = fsz(args[0] if args else kw["ap"])
        else:
            o = kw.get("out", args[0] if args else None)
            n = fsz(o)
        if op.eng == "act":
            c = 200 + 0.85 * n + (95 if kw.get("accum_out") is not None else 0)
        elif op.eng == "dve":
            psum = any(getattr(kw.get(k), "space", None) is not None and "PSUM" in str(kw.get(k).space).upper() for k in ("in_", "in0", "in1"))
            c = 65 + 1.05 * n + (65 if psum else 0)
        else:
            c = 150 + 1.8 * n
        return c, c + 60

    def _list_schedule(self, ops, WIN=10 ** 9):
        n = len(ops)
        costs = [self._cost(op) for op in ops]
        uid = [0] * n
        units = []
        open_u = None
        for i, op in enumerate(ops):
            if op.eng == "pe" and op.fn is not None:
                rec = _Recorder()
                op.fn(rec)
                name, args, kw = rec.last
                start = kw.get("start", True) if name == "matmul" else True
                stop = kw.get("stop", True) if name == "matmul" else True
                if start or open_u is None:
                    units.append([i])
                    open_u = len(units) - 1
                else:
                    units[open_u].append(i)
                uid[i] = open_u
                if stop:
                    open_u = None
            else:
                units.append([i])
                uid[i] = len(units) - 1
        nu = len(units)
        udeps = [set() for _ in range(nu)]
        for i, op in enumerate(ops):
            u = uid[i]
            for j in op.deps:
                if uid[j] != u:
                    udeps[u].add(uid[j])
        succ = [[] for _ in range(nu)]
        indeg = [len(d) for d in udeps]
        for u, d in enumerate(udeps):
            for v in d:
                succ[v].append(u)
        ueng = [ops[m[0]].eng for m in units]
        uocc = [sum(costs[i][0] for i in m) for m in units]
        ulat = [uocc[u] - costs[m[-1]][0] + costs[m[-1]][1] for u, m in enumerate(units)]
        rt = [0.0] * nu
        fin = [0.0] * nu
        free = {e: 0.0 for e in self.ENGS}
        ready = {e: [] for e in self.ENGS}
        tail = [0.0] * nu
        if os.environ.get("SCHED_PRIO", "po") == "cp":
            for u in range(nu - 1, -1, -1):
                t = 0.0
                for v in succ[u]:
                    if tail[v] > t:
                        t = tail[v]
                tail[u] = t + ulat[u]
        for u in range(nu):
            if indeg[u] == 0:
                ready[ueng[u]].append(u)
        order = []
        done = 0
        kb_unit = max([uid[i] for i, op in enumerate(ops) if op.fn is None] + [-1])
        while done < nu:
            best = None
            for e in self.ENGS:
                lst = ready[e]
                if not lst:
                    continue
                f = free[e]
                bkey = None
                lo = min(lst)
                for u in lst:
                    if u > lo + WIN:
                        continue
                    stt = rt[u] if rt[u] > f else f
                    key = (stt, -tail[u], u)
                    if bkey is None or key < bkey:
                        bkey = key
                if best is None or bkey < best[0]:
                    best = (bkey, e)
            (stt, _, u), e = best
            ready[e].remove(u)
            free[e] = stt + uocc[u]
            fin[u] = stt + ulat[u]
            order.extend(units[u])
            done += 1
            for s_ in succ[u]:
                indeg[s_] -= 1
                if fin[u] > rt[s_]:
                    rt[s_] = fin[u]
                if indeg[s_] == 0:
                    ready[ueng[s_]].append(s_)
        self.est_ns = max(fin) if fin else 0.0
        self.barrier_ns = [fin[uid[i]] for i, op in enumerate(ops) if op.fn is None]
        busy = {e: 0.0 for e in self.ENGS}
        for i, op in enumerate(ops):
            busy[op.eng] += costs[i][0]
        self.busy_ns = busy
        inv = [0] * n
        for newi, oldi in enumerate(order):
            inv[oldi] = newi
        new_ops = [ops[i] for i in order]
        for op in new_ops:
            op.deps = {inv[j]: raw for j, raw in op.deps.items()}
        return new_ops

    def emit(self, stack):
        nc = self.nc
        ops = self.ops
        allkeys = set()
        for op in ops:
            if op.fn is None:
                op.reads = tuple(allkeys)
                op.writes = tuple(allkeys | {"__barrier__"})
                continue
            allkeys.update(op.reads)
            allkeys.update(op.writes)
        cnt = {e: 0 for e in self.ENGS}
        for op in ops:
            op.pos = cnt[op.eng]
            cnt[op.eng] += 1
        last_w = {}
        readers = {}
        for i, op in enumerate(ops):
            deps = {}
            for k in op.reads:
                j = last_w.get(k)
                if j is not None:
                    deps[j] = True
            for k in op.writes:
                j = last_w.get(k)
                if j is not None and j not in deps:
                    deps[j] = False
                for j in readers.get(k, ()):
                    if j not in deps:
                        deps[j] = False
            deps.pop(i, None)
            op.deps = deps
            for k in op.reads:
                readers.setdefault(k, []).append(i)
            for k in op.writes:
                last_w[k] = i
                readers[k] = []
        if self.reorder:
            mode = os.environ.get("REORDER_MODE", "023")
            mode = {"moe": "3", "all": "0123"}.get(mode, mode)
            segwin = {int(kv.split(":")[0]): int(kv.split(":")[1]) for kv in os.environ.get("SEGWIN", "").split(",") if kv}
            bidx = [i for i, op in enumerate(ops) if op.fn is None]
            bounds = [-1] + bidx + [len(ops)]
            segs = []
            for si_ in range(len(bounds) - 1):
                lo_, hi_ = bounds[si_] + 1, bounds[si_ + 1]
                seg = ops[lo_:hi_]
                if str(si_) in mode and seg:
                    for op in seg:
                        op.deps = {j - lo_: raw for j, raw in op.deps.items() if j >= lo_}
                    seg = self._list_schedule(seg, segwin.get(si_, 10 ** 9))
                segs.extend(seg)
                if hi_ < len(ops):
                    segs.append(ops[hi_])
            ops = segs
            last_w = {}
            readers = {}
            for i, op in enumerate(ops):
                deps = {}
                for k in op.reads:
                    j = last_w.get(k)
                    if j is not None:
                        deps[j] = True
                for k in op.writes:
                    j = last_w.get(k)
                    if j is not None and j not in deps:
                        deps[j] = False
                    for j in readers.get(k, ()):
                        if j not in deps:
                            deps[j] = False
                deps.pop(i, None)
                op.deps = deps
                for k in op.reads:
                    readers.setdefault(k, []).append(i)
                for k in op.writes:
                    last_w[k] = i
                    readers[k] = []
            self.ops = ops
            cnt = {e: 0 for e in self.ENGS}
            for op in ops:
                op.pos = cnt[op.eng]
                cnt[op.eng] += 1
        dma_tot = {}
        for op in ops:
            if op.dma is not None:
                dma_tot[op.dma] = dma_tot.get(op.dma, 0) + 16
                op.dval = dma_tot[op.dma]
        for op in ops:
            if op.dma is not None and op.dma.startswith("G:"):
                op.dval = dma_tot[op.dma]
        need = []
        SAME_ALL = int(os.environ.get("SAME_ALL", 0))
        for i, op in enumerate(ops):
            lst = []
            for j, raw in op.deps.items():
                P = ops[j]
                if P.dma is not None:
                    if not (op.dma == P.dma and P.dma.startswith("G:")):
                        lst.append(j)
                elif P.eng == op.eng:
                    if op.dma is not None or P.fn is None or op.fn is None:
                        lst.append(j)
                    elif raw and (op.pos - P.pos) <= 2 and op.eng != "pe":
                        lst.append(j)
                    elif SAME_ALL and op.eng != "pe" and (op.pos - P.pos) <= SAME_ALL:
                        lst.append(j)
                else:
                    lst.append(j)
            need.append(lst)
            for j in lst:
                if ops[j].dma is None:
                    ops[j].milestone = True
        last_on = {}
        for i, op in enumerate(ops):
            if op.dma is None:
                last_on[op.eng] = i
        for e, i in last_on.items():
            ops[i].milestone = True
        mcnt = {e: 0 for e in self.ENGS}
        for op in ops:
            if op.dma is None and op.milestone:
                op.msem = mcnt[op.eng] // self.CAP
                op.mval = mcnt[op.eng] % self.CAP + 1
                mcnt[op.eng] += 1
        esem = {}
        for e in self.ENGS:
            n = (mcnt[e] + self.CAP - 1) // self.CAP
            esem[e] = [stack.enter_context(nc.semaphore("sem_%s%d" % (e, q))) for q in range(max(n, 1))]
        dsem = {k: stack.enter_context(nc.semaphore("dsem_%d" % n)) for n, k in enumerate(dma_tot)}
        self.n_sems = sum(len(v) for v in esem.values()) + len(dsem)
        seen = {e: {} for e in self.ENGS}
        streams = {e: [] for e in self.ENGS}
        for i, op in enumerate(ops):
            waits = {}
            for j in need[i]:
                P = ops[j]
                if P.dma is not None:
                    s, v = dsem[P.dma], P.dval
                else:
                    s, v = esem[P.eng][P.msem], P.mval
                sid = id(s)
                if seen[op.eng].get(sid, 0) >= v:
                    continue
                if sid not in waits or waits[sid][1] < v:
                    waits[sid] = (s, v)
            for sid, (s, v) in waits.items():
                seen[op.eng][sid] = v
            op.waits = list(waits.values())
            streams[op.eng].append(op)
        final_waits = [(dsem[k], v) for k, v in dma_tot.items()]
        for e, i in last_on.items():
            if e != "sp":
                final_waits.append((esem[e][ops[i].msem], ops[i].mval))
        semval = {}
        ptr = {e: 0 for e in self.ENGS}
        progress = True
        while progress:
            progress = False
            for en in self.ENGS:
                st_ = streams[en]
                while ptr[en] < len(st_):
                    op = st_[ptr[en]]
                    if any(semval.get(id(s_), 0) < v_ for s_, v_ in op.waits):
                        break
                    if op.dma is not None:
                        sm = dsem[op.dma]
                        semval[id(sm)] = semval.get(id(sm), 0) + 16
                    elif op.milestone:
                        sm = esem[en][op.msem]
                        semval[id(sm)] = semval.get(id(sm), 0) + 1
                    ptr[en] += 1
                    progress = True
        stuck = {en: ptr[en] for en in self.ENGS if ptr[en] < len(streams[en])}
        if stuck:
            for en, p in stuck.items():
                op = streams[en][p]
                print("DEADLOCK: engine", en, "op#", p, "dma" if op.dma else "", op.dma, "reads", op.reads[:6], "writes", op.writes[:6],
                      "waits", [(s_.name if hasattr(s_, "name") else str(s_), v_, semval.get(id(s_), 0)) for s_, v_ in op.waits])
            raise RuntimeError("scheduler deadlock")
        block = stack.enter_context(nc.Block())

        def run(e, name):
            for op in streams[name]:
                for s, v in op.waits:
                    e.wait_ge(s, v)
                if op.fn is None:
                    ins = e.nop()
                else:
                    ins = op.fn(e)
                if op.dma is not None:
                    ins.then_inc(dsem[op.dma], 16)
                elif op.milestone:
                    ins.then_inc(esem[name][op.msem], 1)
            if name == "sp":
                for s, v in final_waits:
                    e.wait_ge(s, v)

        @block.tensor
        def _(e):
            run(e, "pe")

        @block.scalar
        def _(e):
            run(e, "act")

        @block.vector
        def _(e):
            run(e, "dve")

        @block.gpsimd
        def _(e):
            run(e, "pool")

        @block.sync
        def _(e):
            run(e, "sp")

        self.stats = {e: len(streams[e]) for e in self.ENGS}


class Buf:
    def __init__(self, h, base, pitch, shape, key):
        self.h = h
        self.base = base
        self.pitch = pitch
        self.shape = list(shape)
        self.key = key
        st = [1] * (len(shape) - 1)
        for i in range(len(shape) - 3, -1, -1):
            st[i] = st[i + 1] * shape[i + 2]
        self.strides = st

    def raw(self, p0, npart, off, dims):
        return bass.AP(self.h, p0 * self.pitch + self.base + off, [[self.pitch, npart]] + [list(d) for d in dims])

    def __getitem__(self, idx):
        if not isinstance(idx, tuple):
            idx = (idx,)
        idx = list(idx) + [slice(None)] * (len(self.shape) - len(idx))
        ps = idx[0]
        if isinstance(ps, int):
            p0, npart = ps, 1
        else:
            p0 = ps.start or 0
            npart = (ps.stop if ps.stop is not None else self.shape[0]) - p0
        off = 0
        dims = []
        for d, ix in enumerate(idx[1:]):
            if isinstance(ix, int):
                off += ix * self.strides[d]
            else:
                a = ix.start or 0
                b = ix.stop if ix.stop is not None else self.shape[d + 1]
                off += a * self.strides[d]
                dims.append([self.strides[d], b - a])
        if not dims:
            dims = [[1, 1]]
        merged = [dims[0]]
        for s, c in dims[1:]:
            ps_, pc = merged[-1]
            if ps_ == s * c:
                merged[-1] = [s, pc * c]
            else:
                merged.append([s, c])
        return self.raw(p0, npart, off, merged)


class Arena:
    def __init__(self, hf, hb, words):
        self.hf = hf
        self.hb = hb
        self.words = words
        self.top = 0
        self.n = 0

    def alloc(self, shape, dt, key=None):
        n = 1
        for s in shape[1:]:
            n *= s
        w = n if dt == F32 else (n + 1) // 2
        w = (w + 7) // 8 * 8
        off = self.top
        self.top += w
        assert self.top <= self.words, "arena overflow %d" % self.top
        self.n += 1
        key = key or ("t%d" % self.n)
        if dt == F32:
            return Buf(self.hf, off, self.words, shape, key)
        return Buf(self.hb, off * 2, self.words * 2, shape, key)

    def mark(self):
        return self.top

    def release(self, m):
        self.top = m


def build(debug=False, phases=3):
    nc = bass.Bass("TRN2", target_bir_lowering=False)

    def din(name, shape):
        return nc.dram_tensor(name, shape, F32, kind="ExternalInput").ap()

    xL = din("xL", [NL, D])
    xO = din("xO", [NL, D])
    cx = din("cx", [CTX, D])
    c_t = din("c_t", [128, 16])
    w_ada = din("w_ada", [D, 6 * D])
    b_ada = din("b_ada", [1, 6 * D])
    gvec = din("gvec", [4, D])
    w_in = din("w_in", [D, DIN])
    b_in = din("b_in", [1, DIN])
    b_gate = din("b_gate", [1, 16])
    ng = din("ng", [1, 512])
    convp = din("convp", [128, 4 * 34])
    w_out = din("w_out", [D, D])
    b_out = din("b_out", [1, D])
    w_r = din("w_r", [D, NE])
    b_r = din("b_r", [1, NE])
    we_gu = din("we_gu", [NE + 1, D, 512])
    we_d = din("we_d", [NE + 1, 256, D])
    consts = din("consts", [128, 5 * 128])
    sel = din("sel", [2, 256])
    out = nc.dram_tensor("out", [NL, D], F32, kind="ExternalOutput").ap()
    x1s = nc.dram_tensor("x1s", [NL, D], F32, kind="Internal").ap()
    hts = nc.dram_tensor("hts", [128, 8 * NL], BF16, kind="Internal").ap()
    tmpbs = nc.dram_tensor("tmpbs", [NCH, 64, 4 * 130], BF16, kind="Internal").ap()
    dbg_out = {}

    S = Sched(nc)
    with ExitStack() as st:
        hf = st.enter_context(nc.sbuf_tensor("arena", [128, ARENA_WORDS], F32))
        hb = hf.bitcast(BF16)
        AR = Arena(hf, hb, ARENA_WORDS)
        psf = [st.enter_context(nc.psum_tensor("ps%d" % i, [128, 512], F32)) for i in range(8)]
        psb = [p.bitcast(BF16) for p in psf]
        PF = [Buf(psf[i], 0, 512, [128, 512], "ps%d" % i) for i in range(8)]
        PB = [Buf(psb[i], 0, 1024, [128, 1024], "ps%d" % i) for i in range(8)]
        pctr = [0]

        def bank():
            if os.environ.get("BANK_RR"):
                i = pctr[0] % 8
                pctr[0] += 1
                return i
            i = min(range(8), key=lambda b: S.ps_touch.get("ps%d" % b, -1))
            S.ps_touch["ps%d" % i] = len(S.ops) + 0.5
            return i

        def dbg(name, buf, shape2d, apfn):
            if not debug:
                return
            t = nc.dram_tensor("dbg_" + name, list(shape2d), F32, kind="ExternalOutput").ap()
            dbg_out[name] = shape2d
            S.dma("pool", lambda e: e.dma_start(out=t, in_=apfn()), "dbg_" + name, r=[buf.key])

        IDB = AR.alloc([128, 128], BF16, "IDB")
        ONESB = AR.alloc([128, 128], BF16, "ONESB")
        CF = AR.alloc([128, 5, 128], F32, "CF")
        WTS = AR.alloc([128, NCH, NE + 1], F32, "WTS")
        A4x = AR.alloc([128, D], F32, "A4x")
        persist_mark = AR.mark()

        S.dma("sp", lambda e: e.dma_start(out=CF[:], in_=consts), "G:c0", w=["CF"])
        S.dma("pool", lambda e: e.dma_start(out=IDB[:], in_=consts[:, 0:128]), "G:c1", w=["IDB"])
        S.dma("pool", lambda e: e.dma_start(out=ONESB[:], in_=consts[:, 384:512]), "G:c1", w=["ONESB"])
        IDF = lambda: CF[:, 0, :]
        MA = lambda: CF[:, 1, :]
        MB = lambda: CF[:, 2, :]
        ONESF = lambda: CF[:, 3, :]
        ONES512 = lambda: CF[:, 4, :]
        S.pool(lambda e: e.memset(WTS[:], 1.0), w=["WTS"])

        WIN = AR.alloc([128, 8, DIN], BF16, "WIN")
        WOUT = AR.alloc([128, 8, D], BF16, "WOUT")
        BINR = AR.alloc([1, DIN], BF16, "BINR")
        BGR = AR.alloc([1, 16], BF16, "BGR")
        BOUTR = AR.alloc([1, D], BF16, "BOUTR")
        ONER = AR.alloc([1, 512], BF16, "ONER")
        A1x = AR.alloc([128, D], BF16, "A1x")
        S1x = AR.alloc([128, D], BF16, "S1x")
        A1c = AR.alloc([128, D], BF16, "A1c")
        S1c = AR.alloc([128, D], BF16, "S1c")
        A2x = AR.alloc([128, D], F32, "A2x")
        A3x = AR.alloc([128, D], F32, "A3x")
        S3x = AR.alloc([128, D], F32, "S3x")
        NGB = AR.alloc([128, 512], F32, "NGB")
        CONVP = AR.alloc([128, 4, 34], F32, "CONVP")
        WRF = AR.alloc([128, 8, NE], F32, "WRF")
        BRB = AR.alloc([128, NE], F32, "BRB")

        S.dma("pool", lambda e: e.dma_start(out=WIN[:], in_=w_in.rearrange("(k p) n -> p k n", p=128)), "G:wl", w=["WIN"])
        S.dma("pool", lambda e: e.dma_start(out=WOUT[:], in_=w_out.rearrange("(k p) n -> p k n", p=128)), "G:wl", w=["WOUT"])
        S.dma("pool", lambda e: e.dma_start(out=BINR[:], in_=b_in), "G:wl", w=["BINR"])
        S.dma("pool", lambda e: e.dma_start(out=BGR[:], in_=b_gate), "G:wl", w=["BGR"])
        S.dma("pool", lambda e: e.dma_start(out=BOUTR[:], in_=b_out), "G:wl", w=["BOUTR"])
        S.dma("sp", lambda e: e.dma_start(out=NGB[:], in_=ng.partition_broadcast(128)), "G:c0", w=["NGB"])
        S.dma("sp", lambda e: e.dma_start(out=BRB[:], in_=b_r.partition_broadcast(128)), "G:c0", w=["BRB"])
        S.dma("sp", lambda e: e.dma_start(out=CONVP[:], in_=convp), "G:c0", w=["CONVP"])
        S.dma("sp", lambda e: e.dma_start(out=WRF[:], in_=w_r.rearrange("(k p) n -> p k n", p=128)), "G:c0", w=["WRF"])
        S.pool(lambda e: e.memset(ONER[:], 1.0), w=["ONER"])

        mA = AR.mark()
        CT = AR.alloc([128, 16], F32)
        SC = AR.alloc([128, 16], BF16)
        WAD = AR.alloc([128, 8, 768], BF16)
        MOD = AR.alloc([2, 6 * D], F32)
        BADc = [AR.alloc([2, 512], F32) for _ in range(2)]
        GV = AR.alloc([2, 4, D], F32)
        SEL = AR.alloc([2, 256], F32)
        S.dma("sp", lambda e: e.dma_start(out=CT[:], in_=c_t), "G:a0", w=[CT.key])
        S.dma("sp", lambda e: e.dma_start(out=SEL[:], in_=sel), "G:a0", w=[SEL.key])
        for gi in range(4):
            S.dma("sp", lambda e, gi=gi: e.dma_start(out=GV[:, gi, :], in_=gvec[gi:gi + 1, :].partition_broadcast(2)), "G:a0", w=[GV.key])
        S.act(lambda e: e.activation(out=SC[:], in_=CT[:], func=AF.Silu), r=[CT.key], w=[SC.key])
        for piece in range(8):
            S.dma("pool", lambda e, piece=piece: e.dma_start(
                out=WAD[:], in_=w_ada[:, piece * 768:(piece + 1) * 768].rearrange("(k p) n -> p k n", p=128)),
                "wad", w=[WAD.key])
            for nn in range(2):
                bk = bank()
                c0 = piece * 768 + nn * 384
                bd = BADc[(piece * 2 + nn) % 2]
                S.dma("sp", lambda e, bd=bd, c0=c0: e.dma_start(out=bd[:, 0:384], in_=b_ada[:, c0:c0 + 384].partition_broadcast(2)),
                      "bad%d" % ((piece * 2 + nn) % 2), w=[bd.key])
                for k in range(8):
                    S.pe(lambda e, bk=bk, k=k, nn=nn: e.matmul(PF[bk][0:2, 0:384], lhsT=SC.raw(0, 128, k, [[8, 2]]),
                                                               rhs=WAD[:, k, nn * 384:(nn + 1) * 384], start=(k == 0), stop=(k == 7)),
                         r=[SC.key, WAD.key], w=[PF[bk].key])
                S.dve(lambda e, bk=bk, c0=c0, bd=bd: e.tensor_tensor(out=MOD[:, c0:c0 + 384], in0=PF[bk][0:2, 0:384], in1=bd[:, 0:384], op=ALU.add),
                      r=[PF[bk].key, bd.key], w=[MOD.key])
        dbg("MOD", MOD, [2, 6 * D], lambda: MOD[:])
        dbg("SC", SC, [128, 16], lambda: SC[:])
        dbg("GV", GV, [2, 4 * D], lambda: GV[:])
        S.dve(lambda e: e.scalar_tensor_tensor(out=MOD[:, D:2 * D], in0=MOD[:, D:2 * D], scalar=1.0, in1=GV[:, 0, :], op0=ALU.add, op1=ALU.mult),
              r=[MOD.key, GV.key], w=[MOD.key])
        S.dve(lambda e: e.tensor_tensor(out=MOD[:, 2 * D:3 * D], in0=MOD[:, 2 * D:3 * D], in1=GV[:, 1, :], op=ALU.mult), r=[MOD.key, GV.key], w=[MOD.key])
        S.dve(lambda e: e.scalar_tensor_tensor(out=MOD[:, 4 * D:5 * D], in0=MOD[:, 4 * D:5 * D], scalar=1.0, in1=GV[:, 2, :], op0=ALU.add, op1=ALU.mult),
              r=[MOD.key, GV.key], w=[MOD.key])
        S.dve(lambda e: e.tensor_tensor(out=MOD[:, 5 * D:6 * D], in0=MOD[:, 5 * D:6 * D], in1=GV[:, 3, :], op=ALU.mult), r=[MOD.key, GV.key], w=[MOD.key])
        for (dst, vi, row) in ((A1x, 1, 0), (S1x, 0, 0), (A1c, 1, 1), (S1c, 0, 1), (A2x, 2, 0), (A3x, 4, 0), (S3x, 3, 0), (A4x, 5, 0)):
            for hh in range(2):
                bk = bank()
                S.pe(lambda e, bk=bk, vi=vi, row=row, hh=hh: e.matmul(PF[bk][:, :], lhsT=SEL[:, row * 128:(row + 1) * 128],
                                                                      rhs=MOD[:, vi * D + hh * 512:vi * D + (hh + 1) * 512], start=True, stop=True),
                     r=[SEL.key, MOD.key], w=[PF[bk].key])
                S.act(lambda e, bk=bk, dst=dst, hh=hh: e.activation(out=dst[:, hh * 512:(hh + 1) * 512], in_=PF[bk][:, :], func=AF.Copy),
                      r=[PF[bk].key], w=[dst.key])
        dbg("A1x", A1x, [128, D], lambda: A1x[:])
        dbg("S3x", S3x, [128, D], lambda: S3x[:])
        dbg("A1c", A1c, [128, D], lambda: A1c[:])
        AR.release(mA)
        S.barrier()
        DIAG = AR.alloc([128, 124, 128], BF16, "DIAG")
        WBs = AR.alloc([128, NCH, 4], F32, "WBs")
        FLBs = AR.alloc([128, NCH, 4], F32, "FLBs")
        CST = [AR.alloc([64, 4, 130], F32, "CST%d" % d) for d in range(2)]
        MST = [AR.alloc([128, 4], F32, "MST%d" % d) for d in range(2)]
        for d in range(2):
            S.pool(lambda e, d=d: e.memset(CST[d][:], 0.0), w=[CST[d].key])
            S.pool(lambda e, d=d: e.memset(MST[d][:], 0.0), w=[MST[d].key])
        for cc in range(4):
            for j in range(31):
                S.pool(lambda e, cc=cc, j=j: e.tensor_scalar(out=DIAG[:, cc * 31 + j, :], in0=CF[:, 0, :],
                                                          scalar1=CONVP[:, cc, j:j + 1], scalar2=None, op0=ALU.mult),
                       r=["CF", "CONVP"], w=["DIAG"])

        NB = 2

        def wtiles(shape, dt, n=NB):
            return [AR.alloc(shape, dt) for _ in range(n)]

        XT = wtiles([128, D], F32, 2)
        JUNK = AR.alloc([128, D], BF16)
        SSQ = wtiles([128, 4], F32)
        TMPF = wtiles([128, D], F32)
        HXB = wtiles([128, D], BF16, 1) * 2
        HXT = wtiles([128, 8, 128], BF16)

        def rms_rstd(i, src_ap_fn, srckeys, width, col):
            q = SSQ[i % NB]
            S.act(lambda e: e.activation(out=JUNK[:, 0:width], in_=src_ap_fn(), func=AF.Square, accum_out=q[:, col:col + 1]),
                  r=srckeys, w=[JUNK.key, q.key])

        def finish_rstd(i, col, n, scale):
            q = SSQ[i % NB]
            S.act(lambda e: e.activation(out=q[:, col:col + n], in_=q[:, col:col + n], func=AF.Sqrt, scale=scale, bias=EPS), r=[q.key], w=[q.key])
            S.dve(lambda e: e.reciprocal(out=q[:, col:col + n], in_=q[:, col:col + n]), r=[q.key], w=[q.key])

        def norm_to_hxT(i, xt, Ab, Sb):
            q = SSQ[i % NB]
            tf = TMPF[i % NB]
            hx = HXB[i % NB]
            hT = HXT[i % NB]
            rms_rstd(i, lambda: xt[:], [xt.key], D, 0)
            finish_rstd(i, 0, 1, 1.0 / D)
            S.dve(lambda e: e.scalar_tensor_tensor(out=tf[:], in0=xt[:], scalar=q[:, 0:1], in1=Ab[:], op0=ALU.mult, op1=ALU.mult),
                  r=[xt.key, q.key, Ab.key], w=[tf.key])
            S.pool(lambda e: e.tensor_tensor(out=hx[:], in0=tf[:], in1=Sb[:], op=ALU.add), r=[tf.key, Sb.key], w=[hx.key])
            bk = bank()
            for k in range(8):
                S.pe(lambda e, k=k, bk=bk: e.transpose(out=PB[bk][:, k * 128:(k + 1) * 128], in_=hx[:, k * 128:(k + 1) * 128], identity=IDB[:]),
                     r=[hx.key, "IDB"], w=[PB[bk].key])
            S.act(lambda e, bk=bk: e.activation(out=hT[:], in_=PB[bk][:, :], func=AF.Copy), r=[PB[bk].key], w=[hT.key])
            return hT

        def proj_tok(hT, bk, c0, n, gate_bias=False):
            for k in range(8):
                S.pe(lambda e, k=k: e.matmul(PF[bk][:, 0:n], lhsT=hT[:, k, :], rhs=WIN[:, k, c0:c0 + n], start=(k == 0), stop=False),
                     r=[hT.key, "WIN"], w=[PF[bk].key])
            S.pe(lambda e: e.matmul(PF[bk][:, 0:n], lhsT=ONER[0:1, 0:128], rhs=BINR[0:1, c0:c0 + n], start=False, stop=not gate_bias),
                 r=["ONER", "BINR"], w=[PF[bk].key])
            if gate_bias:
                S.pe(lambda e: e.matmul(PF[bk][:, n - 16:n], lhsT=ONER[0:1, 0:128], rhs=BGR[0:1, 0:16], start=False, stop=True),
                     r=["ONER", "BGR"], w=[PF[bk].key])

        def proj_feat(hT, bk, off, c0, m, npart=None):
            for k in range(8):
                S.pe(lambda e, k=k: e.matmul(PF[bk][0:m, off:off + 128], lhsT=WIN[:, k, c0:c0 + m], rhs=hT[:, k, :], start=(k == 0), stop=False),
                     r=[hT.key, "WIN"], w=[PF[bk].key])
            S.pe(lambda e: e.matmul(PF[bk][0:m, off:off + 128], lhsT=BINR[0:1, c0:c0 + m], rhs=ONER[0:1, 0:128], start=False, stop=True),
                 r=["ONER", "BINR"], w=[PF[bk].key])

        CQ, CK, CG, CV, CO, CGA, CGB = 0, 256, 512, 528, 1040, 1552, 2064

        GSB = wtiles([128, 16], F32)
        SP = wtiles([128, 4], F32)
        DD = wtiles([128, 4], F32)
        NBT = wtiles([128, 4], F32)
        RR = wtiles([128, 4, 128], BF16)
        MXB = wtiles([128, 4], F32)
        BTOT = wtiles([128, 4], F32)
        WLOC = wtiles([128, 4], F32)
        MM = wtiles([128, 4], F32)
        DM = wtiles([128, 2, 4], F32)
        AE = wtiles([128, 2, 4], F32)
        WS = wtiles([128, 4], F32)
        FL = wtiles([128, 4], F32)
        KSB = wtiles([128, 256], BF16)
        WV = wtiles([128, 4, 130], BF16)
        TMPS = wtiles([64, 4, 130], F32, 1) * 2
        TMPSB = wtiles([64, 4, 130], BF16)
        LE = wtiles([64, 4, 130], F32, 1) * 2

        def gate_pack(i, bkg, d, kcol0=0, gcol0=256):
            j = i % NB
            g, sp, dd, nbt, rr, mxb, btot, wloc = GSB[j], SP[j], DD[j], NBT[j], RR[j], MXB[j], BTOT[j], WLOC[j]
            li0, lf0 = (0, 4) if d == 0 else (8, 12)
            S.dve(lambda e: e.tensor_copy(out=g[:], in_=PF[bkg][:, gcol0:gcol0 + 16]), r=[PF[bkg].key], w=[g.key])
            S.act(lambda e: e.activation(out=sp[:], in_=g[:, lf0:lf0 + 4], func=AF.Exp, scale=-1.0), r=[g.key], w=[sp.key])
            S.act(lambda e: e.activation(out=sp[:], in_=sp[:], func=AF.Ln, bias=1.0), r=[sp.key], w=[sp.key])
            bk = bank()
            msk = MA if d == 0 else MB
            S.pe(lambda e: e.matmul(PF[bk][:, 0:4], lhsT=msk(), rhs=sp[:], start=True, stop=True), r=["CF", sp.key], w=[PF[bk].key])
            S.pe(lambda e: e.matmul(PF[bk][:, 4:8], lhsT=ONESF(), rhs=sp[:], start=True, stop=True), r=["CF", sp.key], w=[PF[bk].key])
            S.dve(lambda e: e.tensor_tensor(out=dd[:], in0=PF[bk][:, 0:4], in1=g[:, li0:li0 + 4], op=ALU.add), r=[PF[bk].key, g.key], w=[dd.key])
            S.act(lambda e: e.activation(out=nbt[:], in_=PF[bk][:, 0:4], func=AF.Copy), r=[PF[bk].key], w=[nbt.key])
            S.act(lambda e: e.activation(out=btot[:], in_=PF[bk][:, 4:8], func=AF.Copy), r=[PF[bk].key], w=[btot.key])
            S.dve(lambda e: e.tensor_tensor(out=rr[:], in0=IDB.raw(0, 128, 0, [[0, 4], [1, 128]]), in1=dd.raw(0, 128, 0, [[1, 4], [0, 128]]), op=ALU.mult),
                  r=["IDB", dd.key], w=[rr.key])
            bk2 = bank()
            S.pe(lambda e: e.matmul(PF[bk2][:, :], lhsT=ONESB[:], rhs=rr[:], start=True, stop=True), r=["ONESB", rr.key], w=[PF[bk2].key])
            S.dve(lambda e: e.tensor_reduce(out=mxb[:], in_=PF[bk2].raw(0, 128, 0, [[128, 4], [1, 128]]), axis=AX.X, op=ALU.max),
                  r=[PF[bk2].key], w=[mxb.key])
            S.dve(lambda e: e.tensor_tensor(out=wloc[:], in0=dd[:], in1=mxb[:], op=ALU.subtract), r=[dd.key, mxb.key], w=[wloc.key])
            S.act(lambda e: e.activation(out=wloc[:], in_=wloc[:], func=AF.Exp), r=[wloc.key], w=[wloc.key])

        def chain(i, d):
            j = i % NB
            mxb, btot, wloc, nbt = MXB[j], BTOT[j], WLOC[j], NBT[j]
            mm, dm, ae, ws, fl = MM[j], DM[j], AE[j], WS[j], FL[j]
            mst = MST[d]
            S.dve(lambda e: e.tensor_tensor(out=mm[:], in0=mst[:], in1=mxb[:], op=ALU.max), r=[mst.key, mxb.key], w=[mm.key])
            S.dve(lambda e: e.tensor_tensor(out=dm[:, 0, :], in0=mst[:], in1=mm[:], op=ALU.subtract), r=[mst.key, mm.key], w=[dm.key])
            S.dve(lambda e: e.tensor_tensor(out=dm[:, 1, :], in0=mxb[:], in1=mm[:], op=ALU.subtract), r=[mxb.key, mm.key], w=[dm.key])
            S.act(lambda e: e.activation(out=ae[:], in_=dm[:], func=AF.Exp), r=[dm.key], w=[ae.key])
            S.dve(lambda e: e.tensor_tensor(out=mst[:], in0=mm[:], in1=btot[:], op=ALU.subtract), r=[mm.key, btot.key], w=[mst.key])
            S.dve(lambda e: e.tensor_tensor(out=ws[:], in0=wloc[:], in1=ae[:, 1, :], op=ALU.mult), r=[wloc.key, ae.key], w=[ws.key])
            S.dve(lambda e: e.tensor_tensor(out=fl[:], in0=nbt[:], in1=mm[:], op=ALU.subtract), r=[nbt.key, mm.key], w=[fl.key])
            S.act(lambda e: e.activation(out=fl[:], in_=fl[:], func=AF.Exp), r=[fl.key], w=[fl.key])

        def state_local(i, bkk, bkv, kcol0=0):
            j = i % NB
            ksb, wv, wloc = KSB[j], WV[j], WLOC[j]
            S.act(lambda e: e.activation(out=ksb[:], in_=PF[bkk][:, kcol0:kcol0 + 256], func=AF.Copy, scale=0.125), r=[PF[bkk].key], w=[ksb.key])
            S.dve(lambda e: e.tensor_tensor(out=wv[:, :, 0:128], in0=PF[bkv].raw(0, 128, 0, [[128, 4], [1, 128]]),
                                            in1=wloc.raw(0, 128, 0, [[1, 4], [0, 128]]), op=ALU.mult),
                  r=[PF[bkv].key, wloc.key], w=[wv.key])
            S.dve(lambda e: e.tensor_copy(out=wv[:, :, 128:129], in_=wloc.raw(0, 128, 0, [[1, 4], [1, 1]])), r=[wloc.key], w=[wv.key])
            b0, b1 = bank(), bank()
            for h in range(4):
                bk = b0 if h < 2 else b1
                S.pe(lambda e, h=h, bk=bk: e.matmul(PF[bk][0:64, (h % 2) * 256:(h % 2) * 256 + 129], lhsT=ksb[:, h * 64:(h + 1) * 64],
                                                    rhs=wv[:, h, 0:129], start=True, stop=True),
                     r=[ksb.key, wv.key], w=[PF[bk].key])
            return b0, b1

        def state_update(i, d, b0, b1):
            j = i % NB
            ae, tm, le = AE[j], TMPS[j], LE[j]
            cst = CST[d]
            S.dve(lambda e: e.tensor_tensor(out=tm[:, :, 0:129], in0=cst[:, :, 0:129], in1=ae.raw(0, 64, 0, [[1, 4], [0, 129]]), op=ALU.mult),
                  r=[cst.key, ae.key], w=[tm.key])
            for hh, bk in enumerate((b0, b1)):
                S.dve(lambda e, hh=hh, bk=bk: e.tensor_tensor(out=le[:, 2 * hh:2 * hh + 2, 0:129], in0=PF[bk].raw(0, 64, 0, [[256, 2], [1, 129]]),
                                                              in1=ae.raw(0, 64, 4 + 2 * hh, [[1, 2], [0, 129]]), op=ALU.mult),
                      r=[PF[bk].key, ae.key], w=[le.key])
            S.pool(lambda e: e.tensor_tensor(out=cst[:, :, 0:129], in0=tm[:, :, 0:129], in1=le[:, :, 0:129], op=ALU.add),
                   r=[tm.key, le.key], w=[cst.key])

        steps = [("c", 0, 0), ("c", 1, 0), ("c", 1, 1), ("c", 0, 1)]
        steps += [("o", c, 1) for c in range(NCH - 1, -1, -1)]
        steps += [("l", c, 1) for c in range(NCH - 1, -1, -1)]
        if phases < 1:
            steps = []
        steps = steps[:int(os.environ.get("NSTEPS", 999))]
        for t_ in (TMPSB[0], TMPSB[1], TMPS[0], LE[0], WV[0], WV[1]):
            S.pool(lambda e, t_=t_: e.memset(t_[:], 0.0), w=[t_.key])

        def state_step(si, src, c, d):
            xt = XT[si % 2]
            srcap = {"c": cx, "o": xO, "l": xL}[src]
            S.dma("sp", lambda e, xt=xt, srcap=srcap, c=c: e.dma_start(out=xt[:], in_=srcap[c * 128:(c + 1) * 128, :]), "xt%d" % (si % 2), w=[xt.key])
            hT = norm_to_hxT(si, xt, A1c if src == "c" else A1x, S1c if src == "c" else S1x)
            bkk, bkv = bank(), bank()
            proj_tok(hT, bkk, CK, 272, gate_bias=True)
            proj_tok(hT, bkv, CV, 512)
            gate_pack(si, bkk, d)
            chain(si, d)
            b0, b1 = state_local(si, bkk, bkv)
            state_update(si, d, b0, b1)
            if src == "l":
                j = si % NB
                LSK = os.environ.get("LSKIP", "")
                if "a" not in LSK:
                    S.act(lambda e, j=j: e.activation(out=TMPSB[j][:, :, 0:129], in_=TMPS[j][:, :, 0:129], func=AF.Copy), r=[TMPS[j].key], w=[TMPSB[j].key])
                if "b" not in LSK:
                    S.dma("sp", lambda e, j=j, c=c: e.dma_start(out=tmpbs[c], in_=TMPSB[j][:]), "tmpb_st%d" % j, r=[TMPSB[j].key], w=["tmpbs%d" % c])
                if "c" not in LSK:
                    S.pool(lambda e, j=j, c=c: e.tensor_copy(out=WBs[:, c, :], in_=WS[j][:]), r=[WS[j].key], w=["WBs"])
                    S.pool(lambda e, j=j, c=c: e.tensor_copy(out=FLBs[:, c, :], in_=FL[j][:]), r=[FL[j].key], w=["FLBs"])
            if debug and si == 1:
                dbg("cstA", CST[0], [64, 520], lambda: CST[0][:])
                dbg("mstA", MST[0], [128, 4], lambda: MST[0][:])
            if debug and si == 3:
                dbg("cstB", CST[1], [64, 520], lambda: CST[1][:])
                dbg("mstB", MST[1], [128, 4], lambda: MST[1][:])

        SPB = [int(v) for v in os.environ.get("SPBAR", "").split(",") if v]
        for si, (src, c, d) in enumerate(steps):
            if si in SPB:
                S.barrier()
            state_step(si, src, c, d)
        if debug:
            dbg("cstBend", CST[1], [64, 520], lambda: CST[1][:])
            dbg("WBs", WBs, [128, NCH * 4], lambda: WBs[:])

        S.barrier()
        one = lambda shape, dt: wtiles(shape, dt, 1) * 2
        SIGO = one([128, 512], F32)
        G2 = SIGO
        VEXT = one([128, 4, 130], BF16)
        QT = one([64, 4, 128], BF16)
        KT = one([64, 4, 128], BF16)
        SPR = one([128, 2, 4, 128], BF16)
        TB = wtiles([64, 4, 130], BF16)
        TA = one([64, 4, 130], BF16)
        DEN = one([128, 8], F32)
        FLAB = one([128, 8], F32)
        HN = one([128, 8, 128], F32)
        HFT = HN
        HH = one([128, 512], F32)
        HSQ = one([128, 512], F32)
        SIGB = HSQ
        MO = one([128, 512], BF16)
        MT = one([128, 4, 128], BF16)
        UPAD = one([128, 4, 2, 94], BF16)
        YB = one([128, 4, 128], F32)
        YSQ = one([128, 4, 128], F32)
        MEAN = one([128, 128], F32)
        M2 = one([128, 128], F32)
        RSTC = one([128, 128], F32)
        CVT = one([128, 4, 128], BF16)
        HF = TMPF
        HFB = HXB
        HT2 = HXT
        SS = one([128, NE], F32)
        SBI = one([128, NE], F32)
        M8 = one([128, 8], F32)
        MSK = one([128, NE], F32)
        RS = one([128, 2], F32)
        for t_ in (TA[0], TB[0], TB[1]):
            S.pool(lambda e, t_=t_: e.memset(t_[:], 0.0), w=[t_.key])
        for j in range(1):
            S.pool(lambda e, j=j: e.memset(UPAD[j][:], 0.0), w=[UPAD[j].key])
            S.pool(lambda e, j=j: e.memset(VEXT[j][:], 1.0), w=[VEXT[j].key])
        print("arena top (mixer):", AR.top)

        nmain = int(os.environ.get('NMAIN', NCH)) if phases >= 2 else 0
        STG = int(os.environ.get('MAINSTAGE', 99))
        def main_chunk(c):
            j = c % NB
            xt = XT[c % 2]
            if os.environ.get("CHUNKBAR"):
                S.barrier()
            S.dma("sp", lambda e, xt=xt, c=c: e.dma_start(out=xt[:], in_=xL[c * 128:(c + 1) * 128, :]), "xt%d" % (c % 2), w=[xt.key])
            S.dma("sp", lambda e, j=j, c=c: e.dma_start(out=TB[j][:], in_=tmpbs[c]), "tb%d" % j, r=["tmpbs%d" % c], w=[TB[j].key])
            hT = norm_to_hxT(c, xt, A1x, S1x)
            bkk, bkv, bko = bank(), bank(), bank()
            proj_tok(hT, bkk, CK, 272, gate_bias=True)
            proj_tok(hT, bkv, CV, 512)
            proj_tok(hT, bko, CO, 512)
            bkq, bkt = bank(), bank()
            for h in range(4):
                proj_feat(hT, bkq, h * 128, CQ + h * 64, 64)
            for h in range(4):
                proj_feat(hT, bkt, h * 128, CK + h * 64, 64)
            if STG < 1:
                return
            S.act(lambda e, j=j: e.activation(out=QT[j][:], in_=PF[bkq][0:64, :], func=AF.Copy), r=[PF[bkq].key], w=[QT[j].key])
            S.act(lambda e, j=j: e.activation(out=KT[j][:], in_=PF[bkt][0:64, :], func=AF.Copy, scale=0.125), r=[PF[bkt].key], w=[KT[j].key])
            S.act(lambda e, j=j: e.activation(out=SIGO[j][:], in_=PF[bko][:, :], func=AF.Sigmoid), r=[PF[bko].key], w=[SIGO[j].key])
            S.dve(lambda e, j=j: e.tensor_copy(out=VEXT[j][:, :, 0:128], in_=PF[bkv].raw(0, 128, 0, [[128, 4], [1, 128]])), r=[PF[bkv].key], w=[VEXT[j].key])
            if STG < 2:
                return
            gate_pack(c, bkk, 0)
            chain(c, 0)
            b0, b1 = state_local(c, bkk, bkv)
            state_update(c, 0, b0, b1)
            S.act(lambda e, j=j: e.activation(out=TA[j][:, :, 0:129], in_=TMPS[j][:, :, 0:129], func=AF.Copy), r=[TMPS[j].key], w=[TA[j].key])
            if STG < 3:
                return
            bks = bank()
            for h in range(4):
                S.pe(lambda e, h=h, j=j: e.matmul(PF[bks][:, h * 128:(h + 1) * 128], lhsT=KT[j][:, h, :], rhs=QT[j][:, h, :], start=True, stop=True),
                     r=[KT[j].key, QT[j].key], w=[PF[bks].key])
            for h in range(4):
                S.dve(lambda e, h=h, j=j: e.scalar_tensor_tensor(out=SPR[j][:, 0, h, :], in0=PF[bks][:, h * 128:(h + 1) * 128], scalar=WS[j][:, h:h + 1],
                                                                 in1=MA(), op0=ALU.mult, op1=ALU.mult),
                      r=[PF[bks].key, WS[j].key, "CF"], w=[SPR[j].key])
                S.dve(lambda e, h=h, j=j, c=c: e.scalar_tensor_tensor(out=SPR[j][:, 1, h, :], in0=PF[bks][:, h * 128:(h + 1) * 128], scalar=WBs[:, c, h:h + 1],
                                                                      in1=MB(), op0=ALU.mult, op1=ALU.mult),
                      r=[PF[bks].key, "WBs", "CF"], w=[SPR[j].key])
            if STG < 4:
                return
            nb_ = [bank() for _ in range(4)]
            for d in range(2):
                for h in range(4):
                    bk = nb_[d * 2 + h // 2]
                    o0 = (h % 2) * 256
                    tt = TA[j] if d == 0 else TB[j]
                    S.pe(lambda e, d=d, h=h, bk=bk, o0=o0, j=j: e.matmul(PF[bk][:, o0:o0 + 129], lhsT=SPR[j][:, d, h, :], rhs=VEXT[j][:, h, 0:129], start=True, stop=False),
                         r=[SPR[j].key, VEXT[j].key], w=[PF[bk].key])
                    S.pe(lambda e, h=h, bk=bk, o0=o0, j=j, tt=tt: e.matmul(PF[bk][:, o0:o0 + 129], lhsT=QT[j][:, h, :], rhs=tt[:, h, 0:129], start=False, stop=True),
                         r=[QT[j].key, tt.key], w=[PF[bk].key])
            if STG < 5:
                return
            S.pool(lambda e, j=j: e.tensor_copy(out=FLAB[j][:, 0:4], in_=FL[j][:]), r=[FL[j].key], w=[FLAB[j].key])
            S.pool(lambda e, j=j, c=c: e.tensor_copy(out=FLAB[j][:, 4:8], in_=FLBs[:, c, :]), r=["FLBs"], w=[FLAB[j].key])
            for q4 in range(4):
                bk = nb_[q4]
                S.dve(lambda e, q4=q4, bk=bk, j=j: e.scalar_tensor_tensor(out=DEN[j][:, 2 * q4:2 * q4 + 2], in0=PF[bk].raw(0, 128, 128, [[256, 2]]), scalar=-1.0,
                                                                         in1=FLAB[j][:, 2 * q4:2 * q4 + 2], op0=ALU.mult, op1=ALU.max),
                      r=[PF[bk].key, FLAB[j].key], w=[DEN[j].key])
                S.dve(lambda e, q4=q4, bk=bk, j=j: e.tensor_tensor(out=DEN[j][:, 2 * q4:2 * q4 + 2], in0=PF[bk].raw(0, 128, 128, [[256, 2]]),
                                                                  in1=DEN[j][:, 2 * q4:2 * q4 + 2], op=ALU.max),
                      r=[PF[bk].key, DEN[j].key], w=[DEN[j].key])
            S.dve(lambda e, j=j: e.reciprocal(out=DEN[j][:], in_=DEN[j][:]), r=[DEN[j].key], w=[DEN[j].key])
            for q4 in range(4):
                bk = nb_[q4]
                S.dve(lambda e, q4=q4, bk=bk, j=j: e.tensor_tensor(out=HN[j][:, 2 * q4:2 * q4 + 2, :], in0=PF[bk].raw(0, 128, 0, [[256, 2], [1, 128]]),
                                                                  in1=DEN[j].raw(0, 128, 2 * q4, [[1, 2], [0, 128]]), op=ALU.mult),
                      r=[PF[bk].key, DEN[j].key], w=[HN[j].key])
            S.pool(lambda e, j=j: e.tensor_tensor(out=HH[j][:], in0=HN[j][:, 0:4, :], in1=HN[j][:, 4:8, :], op=ALU.add), r=[HN[j].key], w=[HH[j].key])
            S.pool(lambda e, j=j: e.tensor_tensor(out=HSQ[j][:], in0=HH[j][:], in1=HH[j][:], op=ALU.mult), r=[HH[j].key], w=[HSQ[j].key])
            S.dve(lambda e, j=j: e.tensor_reduce(out=SSQ[j][:, 0:4], in_=HSQ[j].raw(0, 128, 0, [[128, 4], [1, 128]]), axis=AX.X, op=ALU.add),
                  r=[HSQ[j].key], w=[SSQ[j].key])
            finish_rstd(c, 0, 4, 1.0 / 128)
            S.pool(lambda e, j=j: e.tensor_tensor(out=G2[j][:], in0=SIGO[j][:], in1=NGB[:], op=ALU.mult), r=[SIGO[j].key, "NGB"], w=[G2[j].key])
            S.dve(lambda e, j=j: e.tensor_tensor(out=HH[j][:], in0=HH[j].raw(0, 128, 0, [[128, 4], [1, 128]]), in1=SSQ[j].raw(0, 128, 0, [[1, 4], [0, 128]]), op=ALU.mult),
                  r=[HH[j].key, SSQ[j].key], w=[HH[j].key])
            S.pool(lambda e, j=j: e.tensor_tensor(out=MO[j][:], in0=HH[j][:], in1=G2[j][:], op=ALU.mult), r=[HH[j].key, G2[j].key], w=[MO[j].key])
            bk = bank()
            for k in range(4):
                S.pe(lambda e, k=k, bk=bk, j=j: e.transpose(out=PB[bk][:, k * 128:(k + 1) * 128], in_=MO[j][:, k * 128:(k + 1) * 128], identity=IDB[:]),
                     r=[MO[j].key, "IDB"], w=[PB[bk].key])
            S.act(lambda e, bk=bk, j=j: e.activation(out=MT[j][:], in_=PB[bk][:, 0:512], func=AF.Copy), r=[PB[bk].key], w=[MT[j].key])
            if STG < 6:
                return
            bka, bkb = bank(), bank()
            for cc in range(4):
                proj_feat(hT, bka, cc * 128, CGA + cc * 128, 128)
            for cc in range(4):
                proj_feat(hT, bkb, cc * 128, CGB + cc * 128, 128)
            S.act(lambda e, j=j: e.activation(out=SIGB[j][:], in_=PF[bkb][:, :], func=AF.Sigmoid), r=[PF[bkb].key], w=[SIGB[j].key])
            for cc in range(4):
                S.dve(lambda e, cc=cc, j=j: e.tensor_tensor(out=UPAD[j].raw(0, 128, cc * 188 + 15, [[94, 2], [1, 64]]), in0=PF[bka].raw(0, 128, cc * 128, [[64, 2], [1, 64]]),
                                                           in1=SIGB[j].raw(0, 128, cc * 128, [[64, 2], [1, 64]]), op=ALU.mult),
                      r=[PF[bka].key, SIGB[j].key], w=[UPAD[j].key])
            bky = bank()
            for cc in range(4):
                for t in range(31):
                    S.pe(lambda e, cc=cc, t=t, j=j: e.matmul(PF[bky][:, cc * 128:(cc + 1) * 128], lhsT=DIAG[:, cc * 31 + t, :],
                                                             rhs=UPAD[j].raw(0, 128, cc * 188 + t, [[94, 2], [1, 64]]), start=(t == 0), stop=(t == 30)),
                         r=["DIAG", UPAD[j].key], w=[PF[bky].key])
            for cc in range(4):
                S.act(lambda e, cc=cc, j=j: e.activation(out=YB[j][:, cc, :], in_=PF[bky][:, cc * 128:(cc + 1) * 128], func=AF.Identity, bias=CONVP[:, cc, 31:32]),
                      r=[PF[bky].key, "CONVP"], w=[YB[j].key])
            S.pool(lambda e, j=j: e.tensor_tensor(out=YSQ[j][:], in0=YB[j][:], in1=YB[j][:], op=ALU.mult), r=[YB[j].key], w=[YSQ[j].key])
            bkm = bank()
            for cc in range(4):
                S.pe(lambda e, cc=cc, j=j: e.matmul(PF[bkm][:, 0:128], lhsT=ONES512(), rhs=YB[j][:, cc, :], start=(cc == 0), stop=(cc == 3)),
                     r=["CF", YB[j].key], w=[PF[bkm].key])
            for cc in range(4):
                S.pe(lambda e, cc=cc, j=j: e.matmul(PF[bkm][:, 128:256], lhsT=ONES512(), rhs=YSQ[j][:, cc, :], start=(cc == 0), stop=(cc == 3)),
                     r=["CF", YSQ[j].key], w=[PF[bkm].key])
            S.act(lambda e, j=j: e.activation(out=MEAN[j][:], in_=PF[bkm][:, 0:128], func=AF.Copy), r=[PF[bkm].key], w=[MEAN[j].key])
            S.act(lambda e, j=j: e.activation(out=M2[j][:], in_=PF[bkm][:, 0:128], func=AF.Square), r=[PF[bkm].key], w=[M2[j].key])
            S.dve(lambda e, j=j: e.tensor_tensor(out=RSTC[j][:], in0=PF[bkm][:, 128:256], in1=M2[j][:], op=ALU.subtract), r=[PF[bkm].key, M2[j].key], w=[RSTC[j].key])
            S.act(lambda e, j=j: e.activation(out=RSTC[j][:], in_=RSTC[j][:], func=AF.Sqrt, bias=EPS), r=[RSTC[j].key], w=[RSTC[j].key])
            S.dve(lambda e, j=j: e.reciprocal(out=RSTC[j][:], in_=RSTC[j][:]), r=[RSTC[j].key], w=[RSTC[j].key])
            S.pool(lambda e, j=j: e.tensor_tensor(out=YB[j][:], in0=YB[j][:], in1=MEAN[j].raw(0, 128, 0, [[0, 4], [1, 128]]), op=ALU.subtract),
                   r=[YB[j].key, MEAN[j].key], w=[YB[j].key])
            S.pool(lambda e, j=j: e.tensor_tensor(out=YB[j][:], in0=YB[j][:], in1=RSTC[j].raw(0, 128, 0, [[0, 4], [1, 128]]), op=ALU.mult),
                   r=[YB[j].key, RSTC[j].key], w=[YB[j].key])
            for cc in range(4):
                S.act(lambda e, cc=cc, j=j: e.activation(out=CVT[j][:, cc, :], in_=YB[j][:, cc, :], func=AF.Silu, scale=CONVP[:, cc, 32:33], bias=CONVP[:, cc, 33:34]),
                      r=[YB[j].key, "CONVP"], w=[CVT[j].key])
            if STG < 7:
                return
            by = [bank(), bank()]
            for hh in range(2):
                for k in range(8):
                    src = MT[j] if k < 4 else CVT[j]
                    S.pe(lambda e, hh=hh, k=k, src=src: e.matmul(PF[by[hh]][:, :], lhsT=src[:, k % 4, :], rhs=WOUT[:, k, hh * 512:(hh + 1) * 512], start=(k == 0), stop=False),
                         r=[src.key, "WOUT"], w=[PF[by[hh]].key])
                S.pe(lambda e, hh=hh: e.matmul(PF[by[hh]][:, :], lhsT=ONER[0:1, 0:128], rhs=BOUTR[0:1, hh * 512:(hh + 1) * 512], start=False, stop=True),
                     r=["ONER", "BOUTR"], w=[PF[by[hh]].key])
            if STG < 8:
                return
            rms_rstd(c, lambda: PF[by[0]][:, :], [PF[by[0]].key], 512, 2)
            rms_rstd(c, lambda: PF[by[1]][:, :], [PF[by[1]].key], 512, 3)
            S.dve(lambda e, j=j: e.tensor_tensor(out=SSQ[j][:, 2:3], in0=SSQ[j][:, 2:3], in1=SSQ[j][:, 3:4], op=ALU.add), r=[SSQ[j].key], w=[SSQ[j].key])
            finish_rstd(c, 2, 1, 1.0 / D)
            for hh in range(2):
                S.dve(lambda e, hh=hh, j=j: e.scalar_tensor_tensor(out=TMPF[j][:, hh * 512:(hh + 1) * 512], in0=PF[by[hh]][:, :], scalar=SSQ[j][:, 2:3],
                                                                   in1=A2x[:, hh * 512:(hh + 1) * 512], op0=ALU.mult, op1=ALU.mult),
                      r=[PF[by[hh]].key, SSQ[j].key, "A2x"], w=[TMPF[j].key])
            S.pool(lambda e, j=j, xt=xt: e.tensor_tensor(out=xt[:], in0=xt[:], in1=TMPF[j][:], op=ALU.add), r=[xt.key, TMPF[j].key], w=[xt.key])
            S.dma("sp", lambda e, xt=xt, c=c: e.dma_start(out=x1s[c * 128:(c + 1) * 128, :], in_=xt[:]), "x1st%d" % (c % 2), r=[xt.key], w=["x1s%d" % c])
            if debug and c == 0:
                dbg("hh0", HH[j], [128, 512], lambda: HH[0][:])
                dbg("x1_0", xt, [128, D], lambda: XT[0][:])
                dbg("cvt0", CVT[j], [128, 512], lambda: CVT[0][:])
                dbg("mo0", MO[j], [128, 512], lambda: MO[0][:])
            if STG < 9:
                return
            rms_rstd(c, lambda xt=xt: xt[:], [xt.key], D, 1)
            finish_rstd(c, 1, 1, 1.0 / D)
            S.dve(lambda e, j=j, xt=xt: e.scalar_tensor_tensor(out=TMPF[j][:], in0=xt[:], scalar=SSQ[j][:, 1:2], in1=A3x[:], op0=ALU.mult, op1=ALU.mult),
                  r=[xt.key, SSQ[j].key, "A3x"], w=[TMPF[j].key])
            S.pool(lambda e, j=j: e.tensor_tensor(out=HF[j][:], in0=TMPF[j][:], in1=S3x[:], op=ALU.add), r=[TMPF[j].key, "S3x"], w=[HF[j].key])
            S.pool(lambda e, j=j: e.tensor_copy(out=HFB[j][:], in_=HF[j][:]), r=[HF[j].key], w=[HFB[j].key])
            bk = bank()
            for k in range(8):
                S.pe(lambda e, k=k, bk=bk, j=j: e.transpose(out=PB[bk][:, k * 128:(k + 1) * 128], in_=HFB[j][:, k * 128:(k + 1) * 128], identity=IDB[:]),
                     r=[HFB[j].key, "IDB"], w=[PB[bk].key])
            S.act(lambda e, bk=bk, j=j: e.activation(out=HT2[j][:], in_=PB[bk][:, :], func=AF.Copy), r=[PB[bk].key], w=[HT2[j].key])
            S.dma("sp", lambda e, j=j, c=c: e.dma_start(out=hts.rearrange("p (k t) -> p k t", k=8)[:, :, c * 128:(c + 1) * 128], in_=HT2[j][:]),
                  "htst%d" % j, r=[HT2[j].key], w=["hts%d" % (c // 16)])
            bf = [bank(), bank()]
            for k in range(8):
                S.pe(lambda e, k=k, j=j: e.transpose(out=PF[bf[k // 4]][:, (k % 4) * 128:(k % 4 + 1) * 128], in_=HF[j][:, k * 128:(k + 1) * 128], identity=IDF()),
                     r=[HF[j].key, "CF"], w=[PF[bf[k // 4]].key])
            S.act(lambda e, j=j: e.activation(out=HFT[j][:, 0:4, :], in_=PF[bf[0]][:, :], func=AF.Copy), r=[PF[bf[0]].key], w=[HFT[j].key])
            S.dve(lambda e, j=j: e.tensor_copy(out=HFT[j][:, 4:8, :], in_=PF[bf[1]][:, :]), r=[PF[bf[1]].key], w=[HFT[j].key])
            bkr = bank()
            for k in range(8):
                S.pe(lambda e, k=k, j=j: e.matmul(PF[bkr][:, 0:NE], lhsT=HFT[j][:, k, :], rhs=WRF[:, k, :], start=(k == 0), stop=(k == 7)),
                     r=[HFT[j].key, "WRF"], w=[PF[bkr].key])
            S.act(lambda e, j=j: e.activation(out=SS[j][:], in_=PF[bkr][:, 0:NE], func=AF.Sigmoid), r=[PF[bkr].key], w=[SS[j].key])
            S.dve(lambda e, j=j: e.tensor_tensor(out=SBI[j][:], in0=SS[j][:], in1=BRB[:], op=ALU.add), r=[SS[j].key, "BRB"], w=[SBI[j].key])
            S.dve(lambda e, j=j: e.max(out=M8[j][:], in_=SBI[j][:]), r=[SBI[j].key], w=[M8[j].key])
            S.dve(lambda e, j=j: e.tensor_scalar(out=MSK[j][:], in0=SBI[j][:], scalar1=M8[j][:, 7:8], scalar2=None, op0=ALU.is_ge), r=[SBI[j].key, M8[j].key], w=[MSK[j].key])
            S.dve(lambda e, j=j: e.tensor_tensor(out=MSK[j][:], in0=MSK[j][:], in1=SS[j][:], op=ALU.mult), r=[MSK[j].key, SS[j].key], w=[MSK[j].key])
            S.dve(lambda e, j=j: e.tensor_reduce(out=RS[j][:, 0:1], in_=MSK[j][:], axis=AX.X, op=ALU.add), r=[MSK[j].key], w=[RS[j].key])
            S.dve(lambda e, j=j: e.reciprocal(out=RS[j][:, 1:2], in_=RS[j][:, 0:1]), r=[RS[j].key], w=[RS[j].key])
            S.dve(lambda e, j=j, c=c: e.tensor_scalar(out=WTS[:, c, 0:NE], in0=MSK[j][:], scalar1=RS[j][:, 1:2], scalar2=2.5, op0=ALU.mult, op1=ALU.mult),
                  r=[MSK[j].key, RS[j].key], w=["WTS"])

        for c in range(nmain):
            main_chunk(c)
        if debug:
            dbg("WTS", WTS, [128, NCH * (NE + 1)], lambda: WTS[:])
            t_ = nc.dram_tensor("dbg_x1all", [NL, D], F32, kind="ExternalOutput").ap()
            dbg_out["x1all"] = [NL, D]
            S.dma("sp", lambda e: e.dma_start(out=t_, in_=x1s), "dbg_x1all", r=["x1s%d" % c for c in range(nmain)])

        S.barrier()
        AR.release(persist_mark)
        ACC = AR.alloc([128, 16, D], F32, "ACC")
        HTM = AR.alloc([128, 8, 2048], BF16, "HTM")
        WGU = [AR.alloc([128, 8, 512], BF16) for _ in range(2)]
        WDN = [AR.alloc([128, 2, D], BF16) for _ in range(2)]
        SG = [AR.alloc([128, 256], F32) for _ in range(2)]
        ACT_ = [AR.alloc([128, 256], BF16) for _ in range(2)]
        ACTT = [AR.alloc([128, 2, 128], BF16) for _ in range(2)]
        X1T = [AR.alloc([128, D], F32) for _ in range(2)]
        TF2 = [AR.alloc([128, D], F32) for _ in range(2)]
        JK2 = AR.alloc([128, D], BF16)
        SQ2 = [AR.alloc([128, 2], F32) for _ in range(2)]
        print("arena top (moe):", AR.top)
        nexp = NE + 1 if phases >= 3 else 0
        u = 0
        for half in range(2 if phases >= 3 else 0):
            S.dma("sp", lambda e, half=half: e.dma_start(out=HTM[:], in_=hts.rearrange("p (k t) -> p k t", k=8)[:, :, half * 2048:(half + 1) * 2048]),
                  "htm", r=["hts%d" % half], w=["HTM"])
            for ex in range(nexp):
                wb = ex % 2
                S.dma("pool", lambda e, ex=ex, wb=wb: e.dma_start(out=WGU[wb][:], in_=we_gu[ex].rearrange("(k p) n -> p k n", p=128)), "wgu%d" % wb, w=[WGU[wb].key])
                S.dma("pool", lambda e, ex=ex, wb=wb: e.dma_start(out=WDN[wb][:], in_=we_d[ex].rearrange("(k p) n -> p k n", p=128)), "wdn%d" % wb, w=[WDN[wb].key])
                for i in range(16):
                    ti = half * 16 + i
                    jj = u % 2
                    u += 1
                    bg = bank()
                    for k in range(8):
                        S.pe(lambda e, k=k, i=i, wb=wb, bg=bg: e.matmul(PF[bg][:, :], lhsT=HTM[:, k, i * 128:(i + 1) * 128], rhs=WGU[wb][:, k, :], start=(k == 0), stop=(k == 7)),
                             r=["HTM", WGU[wb].key], w=[PF[bg].key])
                    S.act(lambda e, jj=jj, bg=bg: e.activation(out=SG[jj][:], in_=PF[bg][:, 0:256], func=AF.Silu), r=[PF[bg].key], w=[SG[jj].key])
                    S.dve(lambda e, jj=jj, bg=bg, ti=ti, ex=ex: e.scalar_tensor_tensor(out=ACT_[jj][:], in0=PF[bg][:, 256:512], scalar=WTS[:, ti, ex:ex + 1], in1=SG[jj][:],
                                                                                 op0=ALU.mult, op1=ALU.mult),
                          r=[PF[bg].key, "WTS", SG[jj].key], w=[ACT_[jj].key])
                    bt = bank()
                    for cc in range(2):
                        S.pe(lambda e, cc=cc, jj=jj, bt=bt: e.transpose(out=PB[bt][:, cc * 128:(cc + 1) * 128], in_=ACT_[jj][:, cc * 128:(cc + 1) * 128], identity=IDB[:]),
                             r=[ACT_[jj].key, "IDB"], w=[PB[bt].key])
                    S.act(lambda e, jj=jj, bt=bt: e.activation(out=ACTT[jj][:], in_=PB[bt][:, 0:256], func=AF.Copy), r=[PB[bt].key], w=[ACTT[jj].key])
                    byy = [bank(), bank()]
                    for hh in range(2):
                        for cc in range(2):
                            S.pe(lambda e, hh=hh, cc=cc, jj=jj, wb=wb, byy=byy: e.matmul(PF[byy[hh]][:, :], lhsT=ACTT[jj][:, cc, :], rhs=WDN[wb][:, cc, hh * 512:(hh + 1) * 512],
                                                                                start=(cc == 0), stop=(cc == 1)),
                                 r=[ACTT[jj].key, WDN[wb].key], w=[PF[byy[hh]].key])
                    for hh in range(2):
                        if ex == 0:
                            S.act(lambda e, hh=hh, i=i, byy=byy: e.activation(out=ACC[:, i, hh * 512:(hh + 1) * 512], in_=PF[byy[hh]][:, :], func=AF.Copy),
                                  r=[PF[byy[hh]].key], w=["ACC%d" % i])
                        else:
                            S.dve(lambda e, hh=hh, i=i, byy=byy: e.tensor_tensor(out=ACC[:, i, hh * 512:(hh + 1) * 512], in0=ACC[:, i, hh * 512:(hh + 1) * 512], in1=PF[byy[hh]][:, :], op=ALU.add),
                                  r=[PF[byy[hh]].key, "ACC%d" % i], w=["ACC%d" % i])
            for i in range(16):
                ti = half * 16 + i
                jj = i % 2
                S.dma("sp", lambda e, jj=jj, ti=ti: e.dma_start(out=X1T[jj][:], in_=x1s[ti * 128:(ti + 1) * 128, :]), "x1l%d" % jj, r=["x1s%d" % ti], w=[X1T[jj].key])
                S.act(lambda e, jj=jj, i=i: e.activation(out=JK2[:], in_=ACC[:, i, :], func=AF.Square, accum_out=SQ2[jj][:, 0:1]), r=["ACC%d" % i], w=[JK2.key, SQ2[jj].key])
                S.act(lambda e, jj=jj: e.activation(out=SQ2[jj][:, 0:1], in_=SQ2[jj][:, 0:1], func=AF.Sqrt, scale=1.0 / D, bias=EPS), r=[SQ2[jj].key], w=[SQ2[jj].key])
                S.dve(lambda e, jj=jj: e.reciprocal(out=SQ2[jj][:, 1:2], in_=SQ2[jj][:, 0:1]), r=[SQ2[jj].key], w=[SQ2[jj].key])
                S.dve(lambda e, jj=jj, i=i: e.scalar_tensor_tensor(out=TF2[jj][:], in0=ACC[:, i, :], scalar=SQ2[jj][:, 1:2], in1=A4x[:], op0=ALU.mult, op1=ALU.mult),
                      r=["ACC%d" % i, SQ2[jj].key, "A4x"], w=[TF2[jj].key])
                S.pool(lambda e, jj=jj: e.tensor_tensor(out=TF2[jj][:], in0=TF2[jj][:], in1=X1T[jj][:], op=ALU.add), r=[TF2[jj].key, X1T[jj].key], w=[TF2[jj].key])
                S.dma("sp", lambda e, jj=jj, ti=ti: e.dma_start(out=out[ti * 128:(ti + 1) * 128, :], in_=TF2[jj][:]), "ost%d" % jj, r=[TF2[jj].key])
        S.emit(st)
    print("ops per engine:", S.stats, "sems:", S.n_sems, "est_ms:", getattr(S, "est_ns", 0) / 1e6, "barriers_ms:", [round(x / 1e6, 3) for x in getattr(S, "barrier_ns", [])], "busy_ms:", {k: round(v / 1e6, 2) for k, v in getattr(S, "busy_ns", {}).items()})
    return nc, dbg_out


def _core_inputs(b, s, x, c, ctx, c_ctx, w_ada, b_ada, g_pre_mix, g_post_mix, g_pre_ffn, g_post_ffn, w_in, b_in, b_gate,
                 mlstm_norm_g, conv_w, conv_b, conv_ln_g, conv_ln_b, w_out, b_out, w_router, b_router, shared):
    T = x.shape[1]
    half = T // 2
    if s == 0:
        xl = x[b, :half]
        xo = x[b, half:]
        cxx = ctx[b]
        gperm = list(range(16))
        cw = conv_w[0]
    else:
        xl = x[b, half:][::-1]
        xo = x[b, :half][::-1]
        cxx = ctx[b][::-1]
        gperm = list(range(8, 16)) + list(range(0, 8))
        cw = conv_w[0][::-1]
    cols = np.concatenate([np.arange(0, 256), np.arange(256, 512), 1536 + np.array(gperm), np.arange(512, 1024), np.arange(1024, 1536),
                           np.arange(1552, 2064), np.arange(2064, 2576)])
    c_t = np.concatenate([c[b].reshape(8, 128).T, c_ctx.reshape(8, 128).T], axis=1)
    convp = np.zeros((128, 4, 34), np.float32)
    convp[:, :, 0:31] = cw.reshape(31, 4, 128).transpose(2, 1, 0)
    convp[:, :, 31] = conv_b[0].reshape(4, 128).T
    convp[:, :, 32] = conv_ln_g[0].reshape(4, 128).T
    convp[:, :, 33] = conv_ln_b[0].reshape(4, 128).T
    d = dict(shared)
    d.update(
        xL=np.ascontiguousarray(xl), xO=np.ascontiguousarray(xo), cx=np.ascontiguousarray(cxx),
        c_t=np.ascontiguousarray(c_t, dtype=np.float32),
        w_in=np.ascontiguousarray(w_in[0][:, cols]), b_in=np.ascontiguousarray(b_in[0][cols][None, :]),
        b_gate=np.ascontiguousarray(b_gate[0].reshape(16)[gperm][None, :]),
        convp=np.ascontiguousarray(convp.reshape(128, 136)),
    )
    return d


def _shared_inputs(w_ada, b_ada, g_pre_mix, g_post_mix, g_pre_ffn, g_post_ffn, mlstm_norm_g, w_out, b_out, w_router, b_router,
                   we_gate, we_up, we_down, ws_gate, ws_up, ws_down):
    we_gu = np.empty((NE + 1, D, 512), np.float32)
    we_gu[:NE, :, :256] = we_gate[0]
    we_gu[:NE, :, 256:] = we_up[0]
    we_gu[NE, :, :256] = ws_gate[0]
    we_gu[NE, :, 256:] = ws_up[0]
    we_d = np.empty((NE + 1, 256, D), np.float32)
    we_d[:NE] = we_down[0]
    we_d[NE] = ws_down[0]
    idx = np.arange(128)
    consts = np.zeros((128, 5, 128), np.float32)
    consts[:, 0] = np.eye(128)
    consts[:, 1] = (idx[:, None] <= idx[None, :])
    consts[:, 2] = (idx[:, None] >= idx[None, :])
    consts[:, 3] = 1.0
    consts[:, 4] = 1.0 / 512
    sel = np.zeros((2, 256), np.float32)
    sel[0, :128] = 1.0
    sel[1, 128:] = 1.0
    return dict(
        w_ada=np.ascontiguousarray(w_ada[0]), b_ada=np.ascontiguousarray(b_ada),
        gvec=np.ascontiguousarray(np.stack([g_pre_mix[0], g_post_mix[0], g_pre_ffn[0], g_post_ffn[0]])),
        ng=np.ascontiguousarray(mlstm_norm_g[0].reshape(1, 512)),
        w_out=np.ascontiguousarray(w_out[0]), b_out=np.ascontiguousarray(b_out),
        w_r=np.ascontiguousarray(w_router[0]), b_r=np.ascontiguousarray(b_router),
        we_gu=we_gu, we_d=we_d, consts=consts.reshape(128, 640), sel=sel,
    )


_NC_CACHE = {}


def kernel(x, c, ctx, c_ctx, w_ada, b_ada, g_pre_mix, g_post_mix, g_pre_ffn, g_post_ffn,
           w_in, b_in, b_gate, mlstm_norm_g, conv_w, conv_b, conv_ln_g, conv_ln_b, w_out, b_out,
           w_router, b_router, we_gate, we_up, we_down, ws_gate, ws_up, ws_down):
    args = [np.asarray(a, dtype=np.float32) for a in (x, c, ctx, c_ctx, w_ada, b_ada, g_pre_mix, g_post_mix, g_pre_ffn, g_post_ffn,
                                                     w_in, b_in, b_gate, mlstm_norm_g, conv_w, conv_b, conv_ln_g, conv_ln_b, w_out, b_out,
                                                     w_router, b_router, we_gate, we_up, we_down, ws_gate, ws_up, ws_down)]
    (x, c, ctx, c_ctx, w_ada, b_ada, g_pre_mix, g_post_mix, g_pre_ffn, g_post_ffn, w_in, b_in, b_gate, mlstm_norm_g, conv_w, conv_b,
     conv_ln_g, conv_ln_b, w_out, b_out, w_router, b_router, we_gate, we_up, we_down, ws_gate, ws_up, ws_down) = args
    shared = _shared_inputs(w_ada, b_ada, g_pre_mix, g_post_mix, g_pre_ffn, g_post_ffn, mlstm_norm_g, w_out, b_out, w_router, b_router,
                            we_gate, we_up, we_down, ws_gate, ws_up, ws_down)
    in_maps = []
    for core in range(8):
        b, s = core // 2, core % 2
        in_maps.append(_core_inputs(b, s, x, c, ctx, c_ctx, w_ada, b_ada, g_pre_mix, g_post_mix, g_pre_ffn, g_post_ffn, w_in, b_in, b_gate,
                                    mlstm_norm_g, conv_w, conv_b, conv_ln_g, conv_ln_b, w_out, b_out, w_router, b_router, shared))
    if "nc" not in _NC_CACHE:
        _NC_CACHE["nc"] = build()[0]
    res = run_bass_kernel_spmd(_NC_CACHE["nc"], in_maps, core_ids=list(range(8)))
    B, T = x.shape[0], x.shape[1]
    half = T // 2
    outp = np.empty((B, T, D), np.float32)
    for core in range(8):
        b, s = core // 2, core % 2
        o = np.asarray(res.results[core]["out"])
        if s == 0:
            outp[b, :half] = o
        else:
            outp[b, half:] = o[::-1]
    return outp
```
